# Optimizing a Trainium2 kernel written in Bass

```python
import jax, jax.numpy as jnp
from jax import lax
import numpy as np

D_MODEL = 1024
BATCH = 8
SEQ = 4096
DEPTH = 4

N_MIXERS = 2
N_POOL_LAYERS = (DEPTH + 1) // 2
N_GLA_LAYERS = DEPTH // 2
POOL_WINDOWS = (2, 4, 8, 16)
POOL_GROUPS = len(POOL_WINDOWS)
POOL_GROUP_DIM = D_MODEL // POOL_GROUPS
GLA_HEADS = 4
GLA_KEY_DIM = D_MODEL // 2
GLA_VALUE_DIM = D_MODEL
GLA_HEAD_K = GLA_KEY_DIM // GLA_HEADS
GLA_HEAD_V = GLA_VALUE_DIM // GLA_HEADS
GLA_GATE_RANK = 16
GLA_GATE_TEMP = 16.0
GLA_CHUNK = 64
GLA_IN_DIM = 2 * GLA_KEY_DIM + 2 * GLA_VALUE_DIM + GLA_GATE_RANK
MOE_GROUPS = 8
MOE_EXPERTS_PER_GROUP = 8
MOE_EXPERTS = MOE_GROUPS * MOE_EXPERTS_PER_GROUP
MOE_TOP_E = 2
MOE_D_FF = 384
NORM_EPS = 1e-6

kernel_name = "hybrid_pool_gla_hiermoe_adaln"


def rmsnorm(x, g):
    xf = x.astype(jnp.float32)
    y = xf * lax.rsqrt(jnp.mean(xf * xf, axis=-1, keepdims=True) + NORM_EPS)
    return (y * g.astype(jnp.float32)).astype(x.dtype)


def pool_mixer(h, w, b, scale):
    B, S, _ = h.shape
    hf = h.astype(jnp.float32).reshape(B, S, POOL_GROUPS, POOL_GROUP_DIM)
    cs = jnp.cumsum(hf, axis=1)
    pos = jnp.arange(1, S + 1, dtype=jnp.float32)
    outs = []
    for gi, win in enumerate(POOL_WINDOWS):
        csg = cs[:, :, gi]
        lag = jnp.pad(csg, ((0, 0), (win, 0), (0, 0)))[:, :S]
        mean = (csg - lag) / jnp.minimum(pos, float(win))[None, :, None]
        outs.append(mean - hf[:, :, gi])
    d = jnp.stack(outs, axis=2).astype(h.dtype)
    y = jnp.einsum('bsgc,gcd->bsgd', d, w).reshape(B, S, D_MODEL) + b
    return y * scale


def gla_mixer(h, w_in, w_gate, b_gate, norm_g, w_out):
    B, S, _ = h.shape
    C = GLA_CHUNK
    N = S // C
    proj = h @ w_in
    q, k, v, r, z = jnp.split(
        proj, [GLA_KEY_DIM, 2 * GLA_KEY_DIM, 2 * GLA_KEY_DIM + GLA_VALUE_DIM,
               2 * GLA_KEY_DIM + 2 * GLA_VALUE_DIM], axis=-1)
    g = jax.nn.log_sigmoid((z @ w_gate + b_gate).astype(jnp.float32)) / GLA_GATE_TEMP

    def to_chunks(t, dh):
        return t.astype(jnp.float32).reshape(B, N, C, GLA_HEADS, dh).transpose(0, 3, 1, 2, 4)

    qc = to_chunks(q, GLA_HEAD_K) * (GLA_HEAD_K ** -0.5)
    kc = to_chunks(k, GLA_HEAD_K)
    vc = to_chunks(v, GLA_HEAD_V)
    gc = to_chunks(g, GLA_HEAD_K)
    bcum = jnp.cumsum(gc, axis=3)
    b_last = bcum[:, :, :, -1:, :]
    q_e = qc * jnp.exp(bcum)
    k_e = kc * jnp.exp(-bcum)
    k_dec = kc * jnp.exp(b_last - bcum)
    causal = jnp.tril(jnp.ones((C, C), dtype=bool))
    scores = jnp.where(causal, jnp.einsum('bhnid,bhnjd->bhnij', q_e, k_e), 0.0)
    o_intra = jnp.einsum('bhnij,bhnjv->bhniv', scores, vc)

    def step(state, inp):
        q_n, k_n, v_n, decay_n = inp
        o_n = jnp.einsum('bhid,bhdv->bhiv', q_n, state)
        state = decay_n[..., None] * state + jnp.einsum('bhjd,bhjv->bhdv', k_n, v_n)
        return state, o_n

    xs = (jnp.moveaxis(q_e, 2, 0), jnp.moveaxis(k_dec, 2, 0), jnp.moveaxis(vc, 2, 0),
          jnp.moveaxis(jnp.exp(b_last[:, :, :, 0, :]), 2, 0))
    s0 = jnp.zeros((B, GLA_HEADS, GLA_HEAD_K, GLA_HEAD_V), jnp.float32)
    _, o_inter = lax.scan(step, s0, xs)
    o = o_intra + jnp.moveaxis(o_inter, 0, 2)
    o = o * lax.rsqrt(jnp.mean(o * o, axis=-1, keepdims=True) + NORM_EPS) * norm_g.astype(jnp.float32)
    o = o.transpose(0, 2, 3, 1, 4).reshape(B, S, GLA_VALUE_DIM).astype(h.dtype)
    o = o * jax.nn.silu(r)
    return o @ w_out


def hier_moe(h, w_group, b_group, w_expert, b_expert, w_in, w_out):
    B, S, D = h.shape
    t = h.reshape(-1, D)
    T = t.shape[0]
    p_group = jax.nn.softmax((t @ w_group + b_group).astype(jnp.float32), axis=-1)
    p_g, g_idx = lax.top_k(p_group, 1)
    logits_e = (t @ w_expert + b_expert).astype(jnp.float32).reshape(T, MOE_GROUPS, MOE_EXPERTS_PER_GROUP)
    logits_sel = jnp.take_along_axis(logits_e, g_idx[:, :, None], axis=1)[:, 0]
    p_e = jax.nn.softmax(logits_sel, axis=-1)
    p_top, e_idx = lax.top_k(p_e, MOE_TOP_E)
    weights = p_g * p_top / jnp.sum(p_top, axis=-1, keepdims=True)
    flat_id = (g_idx * MOE_EXPERTS_PER_GROUP + e_idx).reshape(-1)
    order = jnp.argsort(flat_id)
    token_of = order // MOE_TOP_E
    xs = t[token_of]
    group_sizes = jnp.bincount(flat_id, length=MOE_EXPERTS).astype(jnp.int32)
    gu = lax.ragged_dot(xs, w_in, group_sizes)
    gate, up = jnp.split(gu, 2, axis=-1)
    ys = lax.ragged_dot(jax.nn.silu(gate) * up, w_out, group_sizes)
    ys = ys * weights.reshape(-1)[order][:, None].astype(ys.dtype)
    out = jnp.zeros_like(t).at[token_of].add(ys)
    return out.reshape(B, S, D)


def setup_inputs(seed: int = 0) -> dict:
    key = jax.random.key(seed)
    ks = jax.random.split(key, 22)
    D = D_MODEL
    f32 = jnp.float32
    nrm = lambda k, shape, s: jax.random.normal(k, shape, f32) * s
    return {
        "x": nrm(ks[0], (BATCH, SEQ, D), 1.0),
        "c": nrm(ks[1], (BATCH, D), 1.0),
        "norm_gain": 1.0 + nrm(ks[2], (DEPTH, 2, D), 0.1),
        "w_mod": nrm(ks[3], (DEPTH, D, 6 * D), 0.5 * D ** -0.5),
        "b_mod": nrm(ks[4], (DEPTH, 6 * D), 0.02),
        "pool_w": nrm(ks[5], (N_POOL_LAYERS, POOL_GROUPS, POOL_GROUP_DIM, POOL_GROUP_DIM), POOL_GROUP_DIM ** -0.5),
        "pool_b": nrm(ks[6], (N_POOL_LAYERS, D), 0.02),
        "pool_scale": 1.0 + nrm(ks[7], (N_POOL_LAYERS, D), 0.1),
        "gla_w_in": nrm(ks[8], (N_GLA_LAYERS, D, GLA_IN_DIM), D ** -0.5),
        "gla_w_gate": nrm(ks[9], (N_GLA_LAYERS, GLA_GATE_RANK, GLA_KEY_DIM), GLA_GATE_RANK ** -0.5),
        "gla_b_gate": nrm(ks[10], (N_GLA_LAYERS, GLA_KEY_DIM), 0.1),
        "gla_norm_g": 1.0 + nrm(ks[11], (N_GLA_LAYERS, GLA_HEAD_V), 0.1),
        "gla_w_out": nrm(ks[12], (N_GLA_LAYERS, GLA_VALUE_DIM, D), GLA_VALUE_DIM ** -0.5),
        "moe_w_group": nrm(ks[13], (DEPTH, D, MOE_GROUPS), D ** -0.5),
        "moe_b_group": nrm(ks[14], (DEPTH, MOE_GROUPS), 0.01),
        "moe_w_expert": nrm(ks[15], (DEPTH, D, MOE_EXPERTS), D ** -0.5),
        "moe_b_expert": nrm(ks[16], (DEPTH, MOE_EXPERTS), 0.01),
        "moe_w_in": nrm(ks[17], (DEPTH, MOE_EXPERTS, D, 2 * MOE_D_FF), D ** -0.5),
        "moe_w_out": nrm(ks[18], (DEPTH, MOE_EXPERTS, MOE_D_FF, D), MOE_D_FF ** -0.5),
        "final_norm_g": 1.0 + nrm(ks[19], (D,), 0.1),
    }


def reference(x, c, norm_gain, w_mod, b_mod, pool_w, pool_b, pool_scale,
              gla_w_in, gla_w_gate, gla_b_gate, gla_norm_g, gla_w_out,
              moe_w_group, moe_b_group, moe_w_expert, moe_b_expert, moe_w_in, moe_w_out,
              final_norm_g):
    for i in range(DEPTH):
        mod = (jax.nn.silu(c) @ w_mod[i] + b_mod[i])[:, None, :]
        shift1, scale1, gate1, shift2, scale2, gate2 = jnp.split(mod, 6, axis=-1)
        h = rmsnorm(x, norm_gain[i, 0]) * (1.0 + scale1) + shift1
        j = i // N_MIXERS
        if i % N_MIXERS == 0:
            y = pool_mixer(h, pool_w[j], pool_b[j], pool_scale[j])
        else:
            y = gla_mixer(h, gla_w_in[j], gla_w_gate[j], gla_b_gate[j], gla_norm_g[j], gla_w_out[j])
        x = x + gate1 * y
        h = rmsnorm(x, norm_gain[i, 1]) * (1.0 + scale2) + shift2
        x = x + gate2 * hier_moe(h, moe_w_group[i], moe_b_group[i], moe_w_expert[i], moe_b_expert[i],
                                 moe_w_in[i], moe_w_out[i])
    return rmsnorm(x, final_norm_g)
```

```python
import numpy as np
import concourse.bass as bass
import concourse.mybir as mybir
from concourse.bass_utils import run_bass_kernel_spmd

F32 = mybir.dt.float32
BF16 = mybir.dt.bfloat16
F32R = mybir.dt.float32r
I32 = mybir.dt.int32
ALU = mybir.AluOpType
ACT = mybir.ActivationFunctionType
AX = mybir.AxisListType

D = 1024
S = 4096
NT = S // 128
DEPTH = 4
EPS = 1e-6
NE = 64
DFF = 384
NSLOT_T = 96
SLOT_R = 256
GIN = 3088


class Buf:
    __slots__ = ("name", "t", "last_w", "readers", "dsem", "multi")

    def __init__(self, name, t, multi=False):
        self.name = name
        self.t = t
        self.last_w = {}
        self.readers = {}
        self.dsem = None
        self.multi = multi

    def __getitem__(self, idx):
        return self.t[idx]


class Prog:
    ENG = ("pe", "act", "dve", "pool", "sp")

    def __init__(self, nc):
        self.nc = nc
        self.eng = {"pe": nc.tensor, "act": nc.scalar, "dve": nc.vector,
                    "pool": nc.gpsimd, "sp": nc.sync}
        self.sems = {}
        self.cnt = {}
        self.seen = {e: {} for e in self.ENG}
        self._stack = []
        self._semcms = []
        self._scoped = []
        self.free_dsems = []
        self.nbuf = 0
        self.nops = 0
        for e in self.ENG:
            self._newsem("E_" + e)

    def _enter(self, cm):
        v = cm.__enter__()
        self._stack.append(cm)
        return v

    def _newsem(self, key):
        cm = self.nc.semaphore("s_" + key)
        s = cm.__enter__()
        self._semcms.append(cm)
        self.sems[key] = s
        self.cnt[key] = 0
        return s

    def sbuf(self, name, shape, dt=F32):
        self.nbuf += 1
        name = "%s_%d" % (name, self.nbuf)
        b = Buf(name, self._enter(self.nc.sbuf_tensor(name, list(shape), dt)))
        self._scoped.append((len(self._stack), b))
        return b

    def psum(self, name, shape, dt=F32):
        return Buf(name, self._enter(self.nc.psum_tensor(name, list(shape), dt)))

    def dram(self, name, shape, dt=F32, kind="Internal", multi=False):
        return Buf(name, self.nc.dram_tensor(name, list(shape), dt, kind=kind), multi)

    def mark(self):
        return len(self._stack)

    def release(self, mark):
        self.barrier()
        while len(self._stack) > mark:
            self._stack.pop().__exit__(None, None, None)
        while self._scoped and self._scoped[-1][0] > mark:
            _, b = self._scoped.pop()
            if b.dsem is not None:
                self.free_dsems.append(b.dsem)
                b.dsem = None

    def barrier(self):
        for e in self.ENG:
            seen = self.seen[e]
            for k, v in self.cnt.items():
                if v > 0 and seen.get(k, 0) < v:
                    seen[k] = v
                    self.eng[e].wait_ge(self.sems[k], v)

    def _deps(self, e, reads, writes, accum):
        need = {}
        for b in reads:
            for k, v in b.last_w.items():
                if need.get(k, 0) < v:
                    need[k] = v
        for b in writes:
            if not (accum or b.multi):
                for k, v in b.last_w.items():
                    if need.get(k, 0) < v:
                        need[k] = v
            for k, v in b.readers.items():
                if need.get(k, 0) < v:
                    need[k] = v
        seen = self.seen[e]
        eng = self.eng[e]
        for k, v in need.items():
            if seen.get(k, 0) < v:
                seen[k] = v
                eng.wait_ge(self.sems[k], v)

    def _mark(self, key, val, reads, writes, accum):
        for b in reads:
            b.readers[key] = val
        for b in writes:
            if accum or b.multi:
                b.last_w[key] = val
            else:
                b.last_w = {key: val}
                b.readers = {}

    def op(self, e, fn, reads=(), writes=(), accum=False):
        self._deps(e, reads, writes, accum)
        key = "E_" + e
        self.cnt[key] += 1
        fn().then_inc(self.sems[key], 1)
        self._mark(key, self.cnt[key], reads, writes, accum)
        self.nops += 1

    def dma(self, e, fn, reads=(), writes=(), sembuf=None, accum=False):
        self._deps(e, reads, writes, accum)
        sb = sembuf if sembuf is not None else (writes[0] if writes else reads[0])
        if sb.dsem is None:
            if self.free_dsems:
                sb.dsem = self.free_dsems.pop()
            else:
                self.nbuf += 1
                sb.dsem = "D%d" % self.nbuf
                self._newsem(sb.dsem)
        key = sb.dsem
        self.cnt[key] += 16
        fn().then_inc(self.sems[key], 16)
        self._mark(key, self.cnt[key], reads, writes, accum)
        self.nops += 1

    def wait_for(self, e, bufs):
        self._deps(e, bufs, (), False)

    def finish(self):
        while self._stack:
            self._stack.pop().__exit__(None, None, None)
        while self._semcms:
            self._semcms.pop().__exit__(None, None, None)


POOL_WINDOWS = (2, 4, 8, 16)


def _consts():
    c = {}
    t = np.arange(128)
    c["ident"] = np.eye(128, dtype=np.float32)
    c["ones"] = np.ones((128, 128), np.float32)
    same = (t[:, None] // 64) == (t[None, :] // 64)
    c["tri2"] = (same & (t[:, None] <= t[None, :])).astype(np.float32)
    c["triu2"] = (same & (t[:, None] > t[None, :])).astype(np.float32)
    c["u128"] = (t[:, None] < t[None, :]).astype(np.float32)
    c["pidx"] = np.tile(np.arange(128, dtype=np.float32)[:, None], (1, 128))
    c["jt2"] = np.tile((256.0 * np.arange(128, dtype=np.float32))[None, :], (128, 1))
    c["jt"] = np.tile((128.0 * np.arange(128, dtype=np.float32))[None, :], (128, 1))
    for w in POOL_WINDOWS:
        d = t[None, :] - t[:, None]
        cur = ((d >= 0) & (d < w)).astype(np.float32) / w - np.eye(128, dtype=np.float32)
        first = ((d >= 0) & (d < w)).astype(np.float32) / np.minimum(t + 1, w)[None, :].astype(np.float32) \
            - np.eye(128, dtype=np.float32)
        dp = t[None, :] + 128 - t[:, None]
        prev = ((dp >= 0) & (dp < w)).astype(np.float32) / w
        c["pwc%d" % w] = cur.astype(np.float32)
        c["pw0%d" % w] = first.astype(np.float32)
        c["pwp%d" % w] = prev.astype(np.float32)
    names = list(c.keys())
    arr = np.concatenate([c[n] for n in names], axis=1).astype(np.float32)
    offs = {n: i * 128 for i, n in enumerate(names)}
    return arr, offs


CONST_ARR, CONST_OFF = _consts()
NCONST = CONST_ARR.shape[1]


def build(nlayers=DEPTH, dbg=False, layers=None, nomoe=False, gstop=99):
    nc = bass.Bass("TRN2", target_bir_lowering=False)
    P = Prog(nc)
    pe, act, dve, pool, sp = nc.tensor, nc.scalar, nc.vector, nc.gpsimd, nc.sync

    def din(name, shape, dt=F32):
        return P.dram(name, shape, dt, kind="ExternalInput")

    x_in = din("x", [S, D])
    c_in = din("c", [1, D])
    norm_gain = din("norm_gain", [DEPTH, 2, D])
    w_mod = din("w_mod", [DEPTH, D, 6 * D])
    b_mod = din("b_mod", [DEPTH, 6 * D])
    pool_w = din("pool_w", [2, 4, 256, 256])
    pool_b = din("pool_b", [2, D])
    pool_scale = din("pool_scale", [2, D])
    gla_w_in = din("gla_w_in", [2, D, GIN])
    gla_w_gate = din("gla_w_gate", [2, 16, 512])
    gla_b_gate = din("gla_b_gate", [2, 512])
    gla_norm_g = din("gla_norm_g", [2, 256])
    gla_w_out = din("gla_w_out", [2, D, D])
    moe_w_group = din("moe_w_group", [DEPTH, D, 8])
    moe_b_group = din("moe_b_group", [DEPTH, 8])
    moe_w_expert = din("moe_w_expert", [DEPTH, D, NE])
    moe_b_expert = din("moe_b_expert", [DEPTH, NE])
    moe_w_in = din("moe_w_in", [DEPTH, NE, D, 2 * DFF])
    moe_w_out = din("moe_w_out", [DEPTH, NE, DFF, D])
    final_norm_g = din("final_norm_g", [1, D])
    consts_in = din("consts", [128, NCONST])

    out = P.dram("out", [S, D], F32, kind="ExternalOutput")
    h2d = P.dram("h2d", [S, D], F32)
    xs_d = P.dram("xs_d", [NSLOT_T * SLOT_R, D], F32, multi=True)
    ys_d = P.dram("ys_d", [NSLOT_T * SLOT_R, D], F32, multi=True)
    XD = [Buf("xd%d" % i, out.t) for i in range(NT)]
    HD = [Buf("hd%d" % i, h2d.t) for i in range(NT)]

    CST = P.sbuf("cst", [128, NCONST])
    MODB = P.sbuf("modb", [128, 6 * D])
    PSB = [P.psum("psb%d" % i, [128, 512]) for i in range(8)]
    P.dma("sp", lambda: sp.dma_start(out=CST[:], in_=consts_in[:]), writes=[CST])

    def C(name):
        o = CONST_OFF[name]
        return CST[:, o:o + 128]

    ident = C("ident")
    ones = C("ones")
    SH1, A1, G1, SH2, A2, G2 = [MODB[:, k * D:(k + 1) * D] for k in range(6)]

    BREG = pool.to_reg(NSLOT_T * SLOT_R - 1)
    BREG2 = pool.to_reg(DEPTH * NE * 128 * 2 - 1)
    BREG3 = pool.to_reg(DEPTH * NE * 128 * 3 - 1)
    bank = [0]

    def nextbank():
        b = PSB[bank[0] % 8]
        bank[0] += 1
        return b

    def bcast_load(dst_buf, dst_ap, row_ap, n):
        P.dma("sp", lambda: sp.dma_start(out=dst_ap, in_=row_ap.partition_broadcast(128)), writes=[dst_buf])

    def rstd_of(xt, junk, ssq, rstd, n, eps=EPS):
        xb, xa = xt
        P.op("dve", lambda: dve.scalar_tensor_tensor(out=junk[:, 0:n], in0=xa, scalar=1.0, in1=xa, op0=ALU.mult,
                                                     op1=ALU.mult, accum_out=ssq[:, 0:1]),
             reads=[xb], writes=[junk, ssq])
        P.op("act", lambda: act.activation(out=rstd[:, 0:1], in_=ssq[:, 0:1], func=ACT.Sqrt, scale=1.0 / n, bias=eps),
             reads=[ssq], writes=[rstd])
        P.op("dve", lambda: dve.reciprocal(out=rstd[:, 0:1], in_=rstd[:, 0:1]), reads=[rstd], writes=[rstd])

    def xsrc(layer, i):
        if first[0]:
            return x_in, x_in[i * 128:(i + 1) * 128, :]
        return XD[i], out[i * 128:(i + 1) * 128, :]

    first = [True]
    for L in (layers if layers is not None else range(nlayers)):
        j2 = L // 2
        is_pool = (L % 2 == 0)
        m0 = P.mark()
        CT = P.sbuf("ct", [128, 8])
        SCB = P.sbuf("scb", [128, 8, 128])
        WM = [P.sbuf("wm%d" % i, [128, 3072]) for i in range(2)]
        BROW = P.sbuf("brow", [1, 6 * D])
        NGB = P.sbuf("ngb", [128, 2 * D])
        with nc.allow_non_contiguous_dma(reason="tiny c column load"):
            P.dma("sp", lambda: sp.dma_start(out=CT[:], in_=c_in[0:1, :].rearrange("a (k p) -> p (a k)", p=128)),
                  writes=[CT])
        P.op("act", lambda: act.activation(out=CT[:], in_=CT[:], func=ACT.Silu), reads=[CT], writes=[CT])
        for kc in range(8):
            P.op("dve", lambda kc=kc: dve.tensor_scalar(out=SCB[:, kc, :], in0=ones, scalar1=CT[:, kc:kc + 1],
                                                        scalar2=None, op0=ALU.mult),
                 reads=[CT, CST], writes=[SCB], accum=True)
        P.dma("sp", lambda: sp.dma_start(out=BROW[:], in_=b_mod[L:L + 1, :]), writes=[BROW])
        bcast_load(NGB, NGB[:, 0:D], norm_gain[L, 0:1, :], D)
        P.dma("sp", lambda: sp.dma_start(out=NGB[:, D:2 * D], in_=norm_gain[L, 1:2, :].partition_broadcast(128)),
              writes=[NGB], accum=True)
        for half in range(2):
            banks = [nextbank() for _ in range(6)]
            for kc in range(8):
                wb = WM[kc % 2]
                P.dma("sp", lambda wb=wb, kc=kc, half=half: sp.dma_start(
                    out=wb[:], in_=w_mod[L, kc * 128:(kc + 1) * 128, half * 3072:(half + 1) * 3072]), writes=[wb])
                for n in range(6):
                    P.op("pe", lambda wb=wb, kc=kc, n=n, b=banks[n]: pe.matmul(
                        b[:], lhsT=SCB[:, kc, :], rhs=wb[:, n * 512:(n + 1) * 512], start=(kc == 0), stop=False),
                        reads=[SCB, wb], writes=[banks[n]], accum=(kc > 0))
            for n in range(6):
                col = half * 3072 + n * 512
                P.op("pe", lambda n=n, b=banks[n], col=col: pe.matmul(
                    b[:], lhsT=ones[0:1, :], rhs=BROW[0:1, col:col + 512], start=False, stop=True),
                    reads=[CST, BROW], writes=[banks[n]], accum=True)
                P.op("act", lambda b=banks[n], col=col: act.copy(out=MODB[:, col:col + 512], in_=b[:]),
                     reads=[banks[n]], writes=[MODB], accum=True)
        P.op("dve", lambda: dve.scalar_tensor_tensor(out=A1, in0=A1, scalar=1.0, in1=NGB[:, 0:D], op0=ALU.add,
                                                     op1=ALU.mult), reads=[MODB, NGB], writes=[MODB])
        P.op("dve", lambda: dve.scalar_tensor_tensor(out=A2, in0=A2, scalar=1.0, in1=NGB[:, D:2 * D], op0=ALU.add,
                                                     op1=ALU.mult), reads=[MODB, NGB], writes=[MODB])
        P.release(m0)

        m0 = P.mark()
        if is_pool:
            PWT = P.sbuf("pwt", [128, 4, 2, 256])
            SGB = P.sbuf("sgb", [128, D])
            BSG = P.sbuf("bsg", [128, D])
            XT = [P.sbuf("xt%d" % i, [128, D]) for i in range(2)]
            HN = [P.sbuf("hn%d" % i, [128, D]) for i in range(2)]
            JK = P.sbuf("jk", [128, D])
            SSQ = P.sbuf("ssq", [128, 1])
            RSTD = P.sbuf("rstd", [128, 1])
            DT = P.sbuf("dt", [128, 8, 128])
            YO = [P.sbuf("yo%d" % i, [128, D]) for i in range(2)]
            P.dma("sp", lambda: sp.dma_start(out=PWT[:], in_=pool_w[j2].rearrange("g (k p) n -> p g k n", p=128)),
                  writes=[PWT])
            bcast_load(SGB, SGB[:], pool_scale[j2:j2 + 1, :], D)
            bcast_load(BSG, BSG[:], pool_b[j2:j2 + 1, :], D)
            P.op("dve", lambda: dve.tensor_tensor(out=SGB[:], in0=SGB[:], in1=G1, op=ALU.mult),
                 reads=[SGB, MODB], writes=[SGB])
            P.op("dve", lambda: dve.tensor_tensor(out=BSG[:], in0=BSG[:], in1=SGB[:], op=ALU.mult),
                 reads=[BSG, SGB], writes=[BSG])
            for i in range(NT):
                xt, hn, hp, yo = XT[i % 2], HN[i % 2], HN[(i + 1) % 2], YO[i % 2]
                sb, sa = xsrc(L, i)
                P.dma("act", lambda xt=xt, sa=sa: act.dma_start(out=xt[:], in_=sa), reads=[sb], writes=[xt])
                rstd_of((xt, xt[:]), JK, SSQ, RSTD, D)
                P.op("dve", lambda xt=xt, hn=hn: dve.scalar_tensor_tensor(
                    out=hn[:], in0=xt[:], scalar=RSTD[:, 0:1], in1=A1, op0=ALU.mult, op1=ALU.mult),
                    reads=[xt, RSTD, MODB], writes=[hn])
                pb = [nextbank(), nextbank()]
                for kc in range(8):
                    w = POOL_WINDOWS[kc // 2]
                    b = pb[kc // 4]
                    o_ap = b[:, (kc % 4) * 128:(kc % 4 + 1) * 128]
                    cur = C(("pw0%d" if i == 0 else "pwc%d") % w)
                    P.op("pe", lambda hn=hn, kc=kc, o_ap=o_ap, cur=cur, i=i: pe.matmul(
                        o_ap, lhsT=hn[:, kc * 128:(kc + 1) * 128], rhs=cur, start=True, stop=(i == 0)),
                        reads=[hn, CST], writes=[b], accum=(kc % 4 > 0))
                    if i > 0:
                        P.op("pe", lambda hp=hp, kc=kc, o_ap=o_ap, w=w: pe.matmul(
                            o_ap, lhsT=hp[:, kc * 128:(kc + 1) * 128], rhs=C("pwp%d" % w), start=False, stop=True),
                            reads=[hp, CST], writes=[b], accum=True)
                for hh in range(2):
                    P.op("act", lambda hh=hh, b=pb[hh]: act.copy(
                        out=DT[:, hh * 4:(hh + 1) * 4, :].rearrange("p a b -> p (a b)"), in_=b[:]),
                        reads=[pb[hh]], writes=[DT], accum=(hh > 0))
                yb = [nextbank(), nextbank()]
                for g in range(4):
                    b = yb[g // 2]
                    o_ap = b[:, (g % 2) * 256:(g % 2 + 1) * 256]
                    for k in range(2):
                        P.op("pe", lambda g=g, k=k, o_ap=o_ap: pe.matmul(
                            o_ap, lhsT=DT[:, 2 * g + k, :], rhs=PWT[:, g, k, :], start=(k == 0), stop=(k == 1)),
                            reads=[DT, PWT], writes=[b], accum=(g % 2 > 0 or k > 0))
                for hh in range(2):
                    sl = slice(hh * 512, (hh + 1) * 512)
                    P.op("dve", lambda hh=hh, sl=sl, yo=yo, b=yb[hh]: dve.tensor_tensor(
                        out=yo[:, sl], in0=b[:], in1=SGB[:, sl], op=ALU.mult),
                        reads=[yb[hh], SGB], writes=[yo], accum=(hh > 0))
                P.op("dve", lambda yo=yo: dve.tensor_tensor(out=yo[:], in0=yo[:], in1=BSG[:], op=ALU.add),
                     reads=[yo, BSG], writes=[yo])
                P.op("dve", lambda yo=yo, xt=xt: dve.tensor_tensor(out=yo[:], in0=yo[:], in1=xt[:], op=ALU.add),
                     reads=[yo, xt], writes=[yo])
                P.dma("sp", lambda yo=yo, i=i: sp.dma_start(out=out[i * 128:(i + 1) * 128, :], in_=yo[:]),
                      reads=[yo], writes=[XD[i]], sembuf=yo)
        else:
            gla_layer(P, nc, L, j2, locals(), gstop)
        P.release(m0)
        first[0] = False

        if not nomoe:
            moe_layer(P, nc, L, locals())

    m0 = P.mark()
    FG = P.sbuf("fg", [128, D])
    XT = [P.sbuf("fxt%d" % i, [128, D]) for i in range(2)]
    YO = [P.sbuf("fyo%d" % i, [128, D]) for i in range(2)]
    JK = P.sbuf("fjk", [128, D])
    SSQ = P.sbuf("fssq", [128, 1])
    RSTD = P.sbuf("frstd", [128, 1])
    bcast_load(FG, FG[:], final_norm_g[0:1, :], D)
    for i in range(NT):
        xt, yo = XT[i % 2], YO[i % 2]
        sb, sa = xsrc(0, i)
        P.dma("act", lambda xt=xt, sa=sa: act.dma_start(out=xt[:], in_=sa), reads=[sb], writes=[xt])
        rstd_of((xt, xt[:]), JK, SSQ, RSTD, D)
        P.op("dve", lambda xt=xt, yo=yo: dve.scalar_tensor_tensor(
            out=yo[:], in0=xt[:], scalar=RSTD[:, 0:1], in1=FG[:], op0=ALU.mult, op1=ALU.mult),
            reads=[xt, RSTD, FG], writes=[yo])
        P.dma("sp", lambda yo=yo, i=i: sp.dma_start(out=out[i * 128:(i + 1) * 128, :], in_=yo[:]),
              reads=[yo], writes=[XD[i]], sembuf=yo)
    P.wait_for("sp", XD)
    P.release(m0)
    P.finish()
    return nc


def moe_layer(P, nc, L, env):
    pe, act, dve, pool, sp = nc.tensor, nc.scalar, nc.vector, nc.gpsimd, nc.sync
    C, ident, ones, nextbank, xsrc, rstd_of = [env[k] for k in ("C", "ident", "ones", "nextbank", "xsrc", "rstd_of")]
    A2, SH2, G2, MODB, CST = [env[k] for k in ("A2", "SH2", "G2", "MODB", "CST")]
    out, h2d, xs_d, ys_d, XD, HD = [env[k] for k in ("out", "h2d", "xs_d", "ys_d", "XD", "HD")]
    moe_w_group, moe_b_group, moe_w_expert, moe_b_expert, moe_w_in, moe_w_out = [
        env[k] for k in ("moe_w_group", "moe_b_group", "moe_w_expert", "moe_b_expert", "moe_w_in", "moe_w_out")]
    NS = NSLOT_T * SLOT_R
    BREG = env["BREG"]
    BREG2 = env["BREG2"]
    BREG3 = env["BREG3"]
    w_in_rows = moe_w_in.t.rearrange("l e (p k) n -> (l e p) (k n)", k=8).rearrange("r (m q) -> (r m) q", m=3)
    w_out_rows = moe_w_out.t.rearrange("l e (p k) n -> (l e p) (k n)", k=3).rearrange("r (m q) -> (r m) q", m=2)

    mR = P.mark()
    SL0 = P.sbuf("sl0", [128, NT], I32)
    SL1 = P.sbuf("sl1", [128, NT], I32)
    W0 = P.sbuf("w0", [128, NT])
    W1 = P.sbuf("w1", [128, NT])
    EJ = P.sbuf("ej", [128, NSLOT_T], I32)
    EJ3 = P.sbuf("ej3", [128, 3, NSLOT_T], I32)
    EJ2 = P.sbuf("ej2", [128, 2, NSLOT_T], I32)

    mA = P.mark()
    WR = P.sbuf("wr", [128, 8, 72])
    BR = P.sbuf("br", [1, 72])
    LG = P.sbuf("lg", [128, NT, 72])
    XT = [P.sbuf("mxt%d" % i, [128, D]) for i in range(2)]
    H2 = [P.sbuf("mh2%d" % i, [128, D]) for i in range(2)]
    H2T = P.sbuf("mh2t", [128, 8, 128])
    JK = P.sbuf("mjk", [128, D])
    SSQ = P.sbuf("mssq", [128, 1])
    RSTD = P.sbuf("mrstd", [128, 1])
    with nc.allow_non_contiguous_dma(reason="small router weight load"):
        P.dma("sp", lambda: sp.dma_start(out=WR[:, :, 0:8], in_=moe_w_group[L].rearrange("(k p) n -> p k n", p=128)),
              writes=[WR])
        P.dma("sp", lambda: sp.dma_start(out=WR[:, :, 8:72], in_=moe_w_expert[L].rearrange("(k p) n -> p k n", p=128)),
              writes=[WR], accum=True)
    P.dma("sp", lambda: sp.dma_start(out=BR[0:1, 0:8], in_=moe_b_group[L:L + 1, :]), writes=[BR])
    P.dma("sp", lambda: sp.dma_start(out=BR[0:1, 8:72], in_=moe_b_expert[L:L + 1, :]), writes=[BR], accum=True)
    for i in range(NT):
        xt, h2 = XT[i % 2], H2[i % 2]
        P.dma("act", lambda xt=xt, i=i: act.dma_start(out=xt[:], in_=out[i * 128:(i + 1) * 128, :]),
              reads=[XD[i]], writes=[xt])
        rstd_of((xt, xt[:]), JK, SSQ, RSTD, D)
        P.op("dve", lambda xt=xt, h2=h2: dve.scalar_tensor_tensor(
            out=h2[:], in0=xt[:], scalar=RSTD[:, 0:1], in1=A2, op0=ALU.mult, op1=ALU.mult),
            reads=[xt, RSTD, MODB], writes=[h2])
        P.op("dve", lambda h2=h2: dve.tensor_tensor(out=h2[:], in0=h2[:], in1=SH2, op=ALU.add),
             reads=[h2, MODB], writes=[h2])
        P.dma("sp", lambda h2=h2, i=i: sp.dma_start(out=h2d[i * 128:(i + 1) * 128, :], in_=h2[:]),
              reads=[h2], writes=[HD[i]], sembuf=h2)
        tb = [nextbank(), nextbank()]
        for kc in range(8):
            b = tb[kc // 4]
            P.op("pe", lambda h2=h2, kc=kc, b=b: pe.transpose(
                b[:, (kc % 4) * 128:(kc % 4 + 1) * 128], h2[:, kc * 128:(kc + 1) * 128], ident),
                reads=[h2, CST], writes=[b], accum=(kc % 4 > 0))
        for hh in range(2):
            P.op("act", lambda hh=hh, b=tb[hh]: act.copy(
                out=H2T[:, hh * 4:(hh + 1) * 4, :].rearrange("p a b -> p (a b)"), in_=b[:]),
                reads=[tb[hh]], writes=[H2T], accum=(hh > 0))
        lb = nextbank()
        for kc in range(8):
            P.op("pe", lambda kc=kc, lb=lb: pe.matmul(lb[:, 0:72], lhsT=H2T[:, kc, :], rhs=WR[:, kc, :],
                                                      start=(kc == 0), stop=False),
                 reads=[H2T, WR], writes=[lb], accum=(kc > 0))
        P.op("pe", lambda lb=lb: pe.matmul(lb[:, 0:72], lhsT=ones[0:1, :], rhs=BR[0:1, :], start=False, stop=True),
             reads=[CST, BR], writes=[lb], accum=True)
        P.op("act", lambda lb=lb, i=i: act.copy(out=LG[:, i, :], in_=lb[:, 0:72]), reads=[lb], writes=[LG], accum=True)

    def T3(name, n, dt=F32):
        return P.sbuf(name, [128, NT, n], dt)

    MG = P.sbuf("r_mg", [128, NT])
    MGM = T3("r_mgm", 8)
    EG = T3("r_eg", 8)
    PG = P.sbuf("r_pg", [128, NT])
    TMP = T3("r_tmp", 64)
    LS = T3("r_ls", 8)
    M1 = P.sbuf("r_m1", [128, NT])
    MK1 = T3("r_mk1", 8)
    LS2 = T3("r_ls2", 8)
    M2 = P.sbuf("r_m2", [128, NT])
    MK2 = T3("r_mk2", 8)
    E2 = P.sbuf("r_e2", [128, NT])
    DEN = P.sbuf("r_den", [128, NT])
    M1F = T3("r_m1f", 64)
    M2F = T3("r_m2f", 64)
    SEL = T3("r_sel", 64)
    CS = T3("r_cs", 64)
    POS = T3("r_pos", 64)
    CNT = P.sbuf("r_cnt", [128, 64])
    PC = P.sbuf("r_pc", [128, 64])
    INC = P.sbuf("r_inc", [128, 64])
    OFF = P.sbuf("r_off", [128, 64])
    SLF = P.sbuf("r_slf", [128, NT])
    EJF = P.sbuf("r_ejf", [128, NSLOT_T])
    TMP2 = P.sbuf("r_tmp2", [128, 64, 32])

    def bc(ap, shape):
        return ap.broadcast_to(shape)

    lg = LG[:, :, 0:8]
    P.op("dve", lambda: dve.tensor_reduce(out=MG[:], in_=lg, axis=AX.X, op=ALU.max), reads=[LG], writes=[MG])
    P.op("dve", lambda: dve.tensor_tensor(out=MGM[:], in0=lg, in1=bc(MG[:].unsqueeze(2), [128, NT, 8]),
                                          op=ALU.is_equal), reads=[LG, MG], writes=[MGM])
    P.op("dve", lambda: dve.tensor_tensor(out=EG[:], in0=lg, in1=bc(MG[:].unsqueeze(2), [128, NT, 8]),
                                          op=ALU.subtract), reads=[LG, MG], writes=[EG])
    P.op("act", lambda: act.activation(out=EG[:], in_=EG[:], func=ACT.Exp), reads=[EG], writes=[EG])
    P.op("dve", lambda: dve.tensor_reduce(out=PG[:], in_=EG[:], axis=AX.X, op=ALU.add), reads=[EG], writes=[PG])
    P.op("dve", lambda: dve.reciprocal(out=PG[:], in_=PG[:]), reads=[PG], writes=[PG])
    le4 = LG[:, :, 8:72].rearrange("p t (g e) -> p t g e", g=8)
    P.op("dve", lambda: dve.tensor_tensor(out=TMP[:].rearrange("p t (g e) -> p t g e", g=8), in0=le4,
                                          in1=bc(MGM[:].unsqueeze(3), [128, NT, 8, 8]), op=ALU.mult),
         reads=[LG, MGM], writes=[TMP])
    P.op("dve", lambda: dve.tensor_reduce(out=LS[:], in_=TMP[:].rearrange("p t (g e) -> p t e g", g=8), axis=AX.X,
                                          op=ALU.add), reads=[TMP], writes=[LS])
    P.op("dve", lambda: dve.tensor_reduce(out=M1[:], in_=LS[:], axis=AX.X, op=ALU.max), reads=[LS], writes=[M1])
    P.op("dve", lambda: dve.tensor_tensor(out=MK1[:], in0=LS[:], in1=bc(M1[:].unsqueeze(2), [128, NT, 8]),
                                          op=ALU.is_equal), reads=[LS, M1], writes=[MK1])
    P.op("dve", lambda: dve.scalar_tensor_tensor(out=LS2[:], in0=MK1[:], scalar=-1e30, in1=LS[:], op0=ALU.mult,
                                                 op1=ALU.add), reads=[MK1, LS], writes=[LS2])
    P.op("dve", lambda: dve.tensor_reduce(out=M2[:], in_=LS2[:], axis=AX.X, op=ALU.max), reads=[LS2], writes=[M2])
    P.op("dve", lambda: dve.tensor_tensor(out=MK2[:], in0=LS2[:], in1=bc(M2[:].unsqueeze(2), [128, NT, 8]),
                                          op=ALU.is_equal), reads=[LS2, M2], writes=[MK2])
    P.op("dve", lambda: dve.tensor_tensor(out=E2[:], in0=M2[:], in1=M1[:], op=ALU.subtract), reads=[M1, M2], writes=[E2])
    P.op("act", lambda: act.activation(out=E2[:], in_=E2[:], func=ACT.Exp), reads=[E2], writes=[E2])
    P.op("dve", lambda: dve.tensor_scalar(out=DEN[:], in0=E2[:], scalar1=1.0, scalar2=None, op0=ALU.add),
         reads=[E2], writes=[DEN])
    P.op("dve", lambda: dve.reciprocal(out=DEN[:], in_=DEN[:]), reads=[DEN], writes=[DEN])
    P.op("dve", lambda: dve.tensor_tensor(out=W0[:], in0=PG[:], in1=DEN[:], op=ALU.mult), reads=[PG, DEN], writes=[W0])
    P.op("dve", lambda: dve.tensor_tensor(out=W1[:], in0=W0[:], in1=E2[:], op=ALU.mult), reads=[W0, E2], writes=[W1])
    for MF, MK in ((M1F, MK1), (M2F, MK2)):
        P.op("dve", lambda MF=MF, MK=MK: dve.tensor_tensor(
            out=MF[:].rearrange("p t (g e) -> p t g e", g=8), in0=bc(MGM[:].unsqueeze(3), [128, NT, 8, 8]),
            in1=bc(MK[:].unsqueeze(2), [128, NT, 8, 8]), op=ALU.mult), reads=[MGM, MK], writes=[MF])
    P.op("dve", lambda: dve.tensor_tensor(out=SEL[:], in0=M1F[:], in1=M2F[:], op=ALU.add), reads=[M1F, M2F], writes=[SEL])
    P.op("dve", lambda: dve.memset(CS[:, 0, :], 0.0), writes=[CS])
    for i in range(1, NT):
        P.op("dve", lambda i=i: dve.tensor_tensor(out=CS[:, i, :], in0=CS[:, i - 1, :], in1=SEL[:, i - 1, :], op=ALU.add),
             reads=[CS, SEL], writes=[CS])
    P.op("dve", lambda: dve.tensor_tensor(out=CNT[:], in0=CS[:, NT - 1, :], in1=SEL[:, NT - 1, :], op=ALU.add),
         reads=[CS, SEL], writes=[CNT])
    for q in range(NT // 8):
        b = nextbank()
        for ii in range(8):
            i = q * 8 + ii
            o_ap = b[:, ii * 64:(ii + 1) * 64]
            P.op("pe", lambda i=i, o_ap=o_ap: pe.matmul(o_ap, lhsT=C("u128"), rhs=SEL[:, i, :], start=True, stop=False),
                 reads=[CST, SEL], writes=[b], accum=(ii > 0))
            P.op("pe", lambda i=i, o_ap=o_ap: pe.matmul(o_ap, lhsT=ones, rhs=CS[:, i, :], start=False, stop=True),
                 reads=[CST, CS], writes=[b], accum=True)
        P.op("act", lambda q=q, b=b: act.copy(out=POS[:, q * 8:(q + 1) * 8, :].rearrange("p a b -> p (a b)"), in_=b[:]),
             reads=[b], writes=[POS], accum=(q > 0))
    b = nextbank()
    P.op("pe", lambda: pe.matmul(b[:, 0:64], lhsT=ones, rhs=CNT[:], start=True, stop=True), reads=[CST, CNT], writes=[b])
    P.op("act", lambda: act.copy(out=CNT[:], in_=b[:, 0:64]), reads=[b], writes=[CNT])
    P.op("dve", lambda: dve.tensor_tensor(out=TMP2[:], in0=bc(CNT[:].unsqueeze(2), [128, 64, 32]),
        in1=bc(C("jt2")[:, 0:32].unsqueeze(1), [128, 64, 32]), op=ALU.is_gt), reads=[CNT, CST], writes=[TMP2])
    P.op("dve", lambda: dve.tensor_reduce(out=PC[:], in_=TMP2[:], axis=AX.X, op=ALU.add), reads=[TMP2], writes=[PC])
    P.op("dve", lambda: dve.tensor_scalar(out=PC[:], in0=PC[:], scalar1=float(SLOT_R), scalar2=None, op0=ALU.mult),
         reads=[PC], writes=[PC])
    P.op("dve", lambda: dve.tensor_tensor_scan(out=INC[:], data0=ones[:, 0:64], data1=PC[:], initial=0.0,
                                               op0=ALU.mult, op1=ALU.add), reads=[CST, PC], writes=[INC])
    P.op("dve", lambda: dve.tensor_tensor(out=OFF[:], in0=INC[:], in1=PC[:], op=ALU.subtract), reads=[INC, PC], writes=[OFF])
    P.op("dve", lambda: dve.tensor_tensor(out=POS[:], in0=POS[:], in1=bc(OFF[:].unsqueeze(1), [128, NT, 64]), op=ALU.add),
         reads=[POS, OFF], writes=[POS])
    for MF, SL in ((M1F, SL0), (M2F, SL1)):
        P.op("dve", lambda MF=MF: dve.tensor_tensor(out=TMP[:], in0=MF[:], in1=POS[:], op=ALU.mult),
             reads=[MF, POS], writes=[TMP])
        P.op("dve", lambda: dve.tensor_reduce(out=SLF[:], in_=TMP[:], axis=AX.X, op=ALU.add), reads=[TMP], writes=[SLF])
        P.op("dve", lambda SL=SL: dve.tensor_copy(out=SL[:], in_=SLF[:]), reads=[SLF], writes=[SL])
    for q in range(NSLOT_T // 32):
        P.op("dve", lambda q=q: dve.tensor_tensor(
            out=TMP[:], in0=bc(INC[:].unsqueeze(1), [128, 32, 64]),
            in1=bc(C("jt2")[:, q * 32:(q + 1) * 32].unsqueeze(2), [128, 32, 64]), op=ALU.is_le),
            reads=[INC, CST], writes=[TMP])
        P.op("dve", lambda q=q: dve.tensor_reduce(out=EJF[:, q * 32:(q + 1) * 32], in_=TMP[:], axis=AX.X, op=ALU.add),
             reads=[TMP], writes=[EJF], accum=(q > 0))
    SKP = P.sbuf("r_skp", [128, NSLOT_T])
    P.op("dve", lambda: dve.tensor_scalar(out=SKP[:], in0=EJF[:], scalar1=63.5, scalar2=1.0e6, op0=ALU.is_gt, op1=ALU.mult),
         reads=[EJF], writes=[SKP])
    P.op("dve", lambda: dve.tensor_tensor(out=EJF[:], in0=EJF[:], in1=SKP[:], op=ALU.add), reads=[EJF, SKP], writes=[EJF])
    P.op("dve", lambda: dve.tensor_scalar(out=EJF[:], in0=EJF[:], scalar1=128.0, scalar2=float(L * NE * 128),
                                          op0=ALU.mult, op1=ALU.add), reads=[EJF], writes=[EJF])
    P.op("dve", lambda: dve.tensor_tensor(out=EJF[:], in0=EJF[:], in1=C("pidx")[:, 0:NSLOT_T], op=ALU.add),
         reads=[EJF, CST], writes=[EJF])
    P.op("dve", lambda: dve.tensor_copy(out=EJ[:], in_=EJF[:]), reads=[EJF], writes=[EJ])
    EJG = P.sbuf("r_ejg", [128, NSLOT_T])
    for m in range(3):
        P.op("dve", lambda m=m: dve.tensor_scalar(out=EJG[:], in0=EJF[:], scalar1=3.0, scalar2=float(m), op0=ALU.mult,
                                                  op1=ALU.add), reads=[EJF], writes=[EJG])
        P.op("dve", lambda m=m: dve.tensor_copy(out=EJ3[:, m, :], in_=EJG[:]), reads=[EJG], writes=[EJ3], accum=(m > 0))
    for m in range(2):
        P.op("dve", lambda m=m: dve.tensor_scalar(out=EJG[:], in0=EJF[:], scalar1=2.0, scalar2=float(m), op0=ALU.mult,
                                                  op1=ALU.add), reads=[EJF], writes=[EJG])
        P.op("dve", lambda m=m: dve.tensor_copy(out=EJ2[:, m, :], in_=EJG[:]), reads=[EJG], writes=[EJ2], accum=(m > 0))

    for i in range(NT):
        h2 = H2[i % 2]
        P.dma("sp", lambda h2=h2, i=i: sp.dma_start(out=h2[:], in_=h2d[i * 128:(i + 1) * 128, :]),
              reads=[HD[i]], writes=[h2])
        for SL in (SL0, SL1):
            P.dma("pool", lambda h2=h2, SL=SL, i=i: pool.indirect_dma_start(
                out=xs_d[:, :], out_offset=bass.IndirectOffsetOnAxis(ap=SL[:, i:i + 1], axis=0), in_=h2[:, :],
                in_offset=None, bounds_check=BREG, oob_is_err=False),
                reads=[h2, SL], writes=[xs_d], sembuf=h2)
    P.release(mA)

    mB = P.mark()
    WI = [P.sbuf("wi%d" % i, [128, 8, 2 * DFF]) for i in range(3)]
    WO = [P.sbuf("wo%d" % i, [128, 3, D]) for i in range(3)]
    XS = [P.sbuf("xs%d" % i, [128, D]) for i in range(3)]
    XT8 = [P.sbuf("xt8%d" % i, [128, 8, 128]) for i in range(2)]
    SG = P.sbuf("sg", [128, DFF])
    HS = [P.sbuf("hs%d" % i, [128, DFF]) for i in range(2)]
    HT = [P.sbuf("ht%d" % i, [128, 3, 128]) for i in range(2)]
    YT = [P.sbuf("yt%d" % i, [128, D]) for i in range(2)]
    PSBl = env["PSB"]
    SUBS = SLOT_R // 128
    NJ = NSLOT_T * SUBS

    def load_w(j):
        wi, wo = WI[j % 3], WO[j % 3]
        for m in range(3):
            P.dma("pool", lambda m=m: pool.indirect_dma_start(
                out=wi[:].rearrange("p k n -> p (k n)")[:, m * 2048:(m + 1) * 2048].bitcast(F32R), out_offset=None,
                in_=w_in_rows, in_offset=bass.IndirectOffsetOnAxis(ap=EJ3[:, m, j:j + 1], axis=0),
                bounds_check=BREG3, oob_is_err=False), reads=[EJ3], writes=[wi], accum=(m > 0))
        for m in range(2):
            P.dma("pool", lambda m=m: pool.indirect_dma_start(
                out=wo[:].rearrange("p k n -> p (k n)")[:, m * 1536:(m + 1) * 1536].bitcast(F32R), out_offset=None,
                in_=w_out_rows, in_offset=bass.IndirectOffsetOnAxis(ap=EJ2[:, m, j:j + 1], axis=0),
                bounds_check=BREG2, oob_is_err=False), reads=[EJ2], writes=[wo], accum=(m > 0))

    def load_x(jj):
        xs = XS[jj % 3]
        P.dma("sp", lambda: sp.dma_start(out=xs[:], in_=xs_d[jj * 128:(jj + 1) * 128, :]), reads=[xs_d], writes=[xs])

    def stC(jj):
        xs, xt8 = XS[jj % 3], XT8[jj % 2]
        tb = [PSBl[0], PSBl[1]]
        for kc in range(8):
            bb = tb[kc // 4]
            P.op("pe", lambda kc=kc, bb=bb: pe.transpose(
                bb[:, (kc % 4) * 128:(kc % 4 + 1) * 128], xs[:].rearrange("s (p k) -> s k p", k=8)[:, kc, :], ident),
                reads=[xs, CST], writes=[bb], accum=(kc % 4 > 0))
        P.op("act", lambda: act.copy(out=xt8[:, 0:4, :].rearrange("p a b -> p (a b)").bitcast(F32R), in_=tb[0][:]),
             reads=[tb[0]], writes=[xt8])
        P.op("dve", lambda: dve.tensor_copy(out=xt8[:, 4:8, :].rearrange("p a b -> p (a b)").bitcast(F32R), in_=tb[1][:]),
             reads=[tb[1]], writes=[xt8], accum=True)

    def stA(jj):
        xt8, hs = XT8[jj % 2], HS[jj % 2]
        wi = WI[(jj // SUBS) % 3]
        gb, ub = PSBl[2], PSBl[3]
        for kc in range(8):
            P.op("pe", lambda kc=kc: pe.matmul(gb[:, 0:DFF], lhsT=xt8[:, kc, :].bitcast(F32R),
                                               rhs=wi[:, kc, 0:DFF].bitcast(F32R), start=(kc == 0), stop=(kc == 7)),
                 reads=[xt8, wi], writes=[gb], accum=(kc > 0))
        for kc in range(8):
            P.op("pe", lambda kc=kc: pe.matmul(ub[:, 0:DFF], lhsT=xt8[:, kc, :].bitcast(F32R),
                                               rhs=wi[:, kc, DFF:2 * DFF].bitcast(F32R), start=(kc == 0), stop=(kc == 7)),
                 reads=[xt8, wi], writes=[ub], accum=(kc > 0))
        P.op("act", lambda: act.activation(out=SG[:], in_=gb[:, 0:DFF], func=ACT.Silu), reads=[gb], writes=[SG])
        P.op("dve", lambda: dve.tensor_tensor(out=hs[:], in0=ub[:, 0:DFF], in1=SG[:], op=ALU.mult),
             reads=[ub, SG], writes=[hs])

    def stB(jj):
        hs, ht = HS[jj % 2], HT[jj % 2]
        hb = PSBl[4]
        for fc in range(3):
            P.op("pe", lambda fc=fc: pe.transpose(hb[:, fc * 128:(fc + 1) * 128],
                                                  hs[:].rearrange("s (p k) -> s k p", k=3)[:, fc, :], ident),
                 reads=[hs, CST], writes=[hb], accum=(fc > 0))
        P.op("act", lambda: act.copy(out=ht[:].rearrange("p a b -> p (a b)").bitcast(F32R), in_=hb[:, 0:384]),
             reads=[hb], writes=[ht])

    def stD(jj):
        ht, yt = HT[jj % 2], YT[jj % 2]
        wo = WO[(jj // SUBS) % 3]
        yb = [PSBl[5], PSBl[6]]
        for hh in range(2):
            for fc in range(3):
                P.op("pe", lambda hh=hh, fc=fc: pe.matmul(
                    yb[hh][:], lhsT=ht[:, fc, :].bitcast(F32R), rhs=wo[:, fc, hh * 512:(hh + 1) * 512].bitcast(F32R),
                    start=(fc == 0), stop=(fc == 2)), reads=[ht, wo], writes=[yb[hh]], accum=(fc > 0))
        P.op("act", lambda: act.copy(out=yt[:, 0:512], in_=yb[0][:]), reads=[yb[0]], writes=[yt])
        P.op("dve", lambda: dve.tensor_copy(out=yt[:, 512:1024], in_=yb[1][:]), reads=[yb[1]], writes=[yt], accum=True)
        P.dma("sp", lambda: sp.dma_start(out=ys_d[jj * 128:(jj + 1) * 128, :], in_=yt[:]),
              reads=[yt], writes=[ys_d], sembuf=yt)

    load_w(0)
    load_w(1)
    load_x(0)
    load_x(1)
    stC(0)
    for jj in range(NJ + 1):
        if jj + 2 < NJ:
            load_x(jj + 2)
        if jj < NJ:
            stA(jj)
        if jj >= 1:
            stB(jj - 1)
        if jj + 1 < NJ:
            stC(jj + 1)
        if jj >= 1:
            stD(jj - 1)
        if jj % SUBS == 0 and jj // SUBS + 2 < NSLOT_T:
            load_w(jj // SUBS + 2)
    P.release(mB)

    mC = P.mark()
    XT = [P.sbuf("cxt%d" % i, [128, D]) for i in range(2)]
    Y0 = [P.sbuf("cy0%d" % i, [128, D]) for i in range(2)]
    Y1 = [P.sbuf("cy1%d" % i, [128, D]) for i in range(2)]
    for i in range(NT):
        xt, y0, y1 = XT[i % 2], Y0[i % 2], Y1[i % 2]
        P.dma("act", lambda xt=xt, i=i: act.dma_start(out=xt[:], in_=out[i * 128:(i + 1) * 128, :]),
              reads=[XD[i]], writes=[xt])
        for yy, SL in ((y0, SL0), (y1, SL1)):
            P.dma("pool", lambda yy=yy, SL=SL, i=i: pool.indirect_dma_start(
                out=yy[:, :], out_offset=None, in_=ys_d[:, :],
                in_offset=bass.IndirectOffsetOnAxis(ap=SL[:, i:i + 1], axis=0), bounds_check=BREG, oob_is_err=False),
                reads=[ys_d, SL], writes=[yy])
        P.op("dve", lambda y0=y0, i=i: dve.tensor_scalar(out=y0[:], in0=y0[:], scalar1=W0[:, i:i + 1], scalar2=None,
                                                         op0=ALU.mult), reads=[y0, W0], writes=[y0])
        P.op("dve", lambda y0=y0, y1=y1, i=i: dve.scalar_tensor_tensor(
            out=y0[:], in0=y1[:], scalar=W1[:, i:i + 1], in1=y0[:], op0=ALU.mult, op1=ALU.add),
            reads=[y0, y1, W1], writes=[y0])
        P.op("dve", lambda y0=y0: dve.tensor_tensor(out=y0[:], in0=y0[:], in1=G2, op=ALU.mult),
             reads=[y0, MODB], writes=[y0])
        P.op("dve", lambda y0=y0, xt=xt: dve.tensor_tensor(out=y0[:], in0=y0[:], in1=xt[:], op=ALU.add),
             reads=[y0, xt], writes=[y0])
        P.dma("sp", lambda y0=y0, i=i: sp.dma_start(out=out[i * 128:(i + 1) * 128, :], in_=y0[:]),
              reads=[y0], writes=[XD[i]], sembuf=y0)
    P.release(mC)
    P.release(mR)


def gla_layer(P, nc, L, j2, env, gstop=99):
    pe, act, dve, pool, sp = nc.tensor, nc.scalar, nc.vector, nc.gpsimd, nc.sync
    C, ident, ones, nextbank, xsrc, rstd_of = [env[k] for k in ("C", "ident", "ones", "nextbank", "xsrc", "rstd_of")]
    A1, SH1, G1, MODB, CST = [env[k] for k in ("A1", "SH1", "G1", "MODB", "CST")]
    out, XD = env["out"], env["XD"]
    gla_w_in, gla_w_gate, gla_b_gate, gla_norm_g, gla_w_out = [
        env[k] for k in ("gla_w_in", "gla_w_gate", "gla_b_gate", "gla_norm_g", "gla_w_out")]
    SC = 128 ** -0.5
    GT = 1.0 / 16.0

    WIN = P.sbuf("g_win", [128, 8, GIN], BF16)
    WOUT = P.sbuf("g_wout", [128, 8, D], BF16)
    IDB = P.sbuf("g_idb", [128, 128], BF16)
    WG = P.sbuf("g_wg", [16, 512])
    BG = P.sbuf("g_bg", [1, 512])
    GN = P.sbuf("g_gn", [128, 256])
    ms = P.mark()
    STG = [P.sbuf("g_stg%d" % i, [128, GIN]) for i in range(2)]
    for kc in range(8):
        st = STG[kc % 2]
        P.dma("sp", lambda st=st, kc=kc: sp.dma_start(out=st[:], in_=gla_w_in[j2, kc * 128:(kc + 1) * 128, :]), writes=[st])
        if kc % 2 == 0:
            P.op("act", lambda st=st, kc=kc: act.copy(out=WIN[:, kc, :], in_=st[:]), reads=[st], writes=[WIN], accum=True)
        else:
            P.op("dve", lambda st=st, kc=kc: dve.tensor_copy(out=WIN[:, kc, :], in_=st[:]), reads=[st], writes=[WIN], accum=True)
    for kc in range(8):
        st = STG[kc % 2]
        P.dma("sp", lambda st=st, kc=kc: sp.dma_start(out=st[:, 0:D], in_=gla_w_out[j2, kc * 128:(kc + 1) * 128, :]), writes=[st])
        if kc % 2 == 0:
            P.op("act", lambda st=st, kc=kc: act.copy(out=WOUT[:, kc, :], in_=st[:, 0:D]), reads=[st], writes=[WOUT], accum=True)
        else:
            P.op("dve", lambda st=st, kc=kc: dve.tensor_copy(out=WOUT[:, kc, :], in_=st[:, 0:D]), reads=[st], writes=[WOUT], accum=True)
    P.release(ms)
    P.op("dve", lambda: dve.tensor_copy(out=IDB[:], in_=ident), reads=[CST], writes=[IDB])
    P.dma("sp", lambda: sp.dma_start(out=WG[:], in_=gla_w_gate[j2]), writes=[WG])
    P.dma("sp", lambda: sp.dma_start(out=BG[:], in_=gla_b_gate[j2:j2 + 1, :]), writes=[BG])
    P.dma("sp", lambda: sp.dma_start(out=GN[:], in_=gla_norm_g[j2:j2 + 1, :].partition_broadcast(128)), writes=[GN])

    XT = [P.sbuf("g_xt%d" % i, [128, D]) for i in range(2)]
    H = [P.sbuf("g_h%d" % i, [128, D]) for i in range(2)]
    HB = P.sbuf("g_hb", [128, D], BF16)
    HT = P.sbuf("g_ht", [128, 8, 128], BF16)
    SSQ = P.sbuf("g_ssq", [128, 1])
    RSTD = P.sbuf("g_rstd", [128, 1])
    QK = P.sbuf("g_qk", [128, D])
    V = P.sbuf("g_v", [128, D])
    SR = P.sbuf("g_sr", [128, D])
    Z = P.sbuf("g_z", [128, 16])
    ZT = P.sbuf("g_zt", [16, 128])
    G = P.sbuf("g_g", [128, 512])
    EB = P.sbuf("g_eb", [128, 4, 128])
    ENB = P.sbuf("g_enb", [128, 4, 128])
    EREV = P.sbuf("g_erev", [128, 512])
    QEZ = P.sbuf("g_qez", [128, 4, 2, 128])
    KE = P.sbuf("g_ke", [128, 4, 128])
    KD0 = P.sbuf("g_kd0", [128, 512])
    KD1 = P.sbuf("g_kd1", [128, 512])
    SA = [P.sbuf("g_sa%d" % h, [128, 256]) for h in range(4)]
    SB = [P.sbuf("g_sb%d" % h, [128, 256]) for h in range(4)]
    SN = [P.sbuf("g_sn%d" % h, [128, 256]) for h in range(4)]
    ST4 = P.sbuf("g_st4", [128, 4, 128])
    SS4 = P.sbuf("g_ss4", [128, 4])
    RS4 = P.sbuf("g_rs4", [128, 4])
    JK = P.sbuf("g_jk", [128, 256])

    P.op("dve", lambda: dve.memset(QEZ[:], 0.0), writes=[QEZ])
    P.op("dve", lambda: dve.memset(KD0[:], 0.0), writes=[KD0])
    P.op("dve", lambda: dve.memset(KD1[:], 0.0), writes=[KD1])
    for h in range(4):
        P.op("dve", lambda h=h: dve.memset(SA[h][:], 0.0), writes=[SA[h]])

    for i in range(NT):
        xt, hh_ = XT[i % 2], H[i % 2]
        sb_, sa_ = xsrc(L, i)
        P.dma("act", lambda xt=xt, sa_=sa_: act.dma_start(out=xt[:], in_=sa_), reads=[sb_], writes=[xt])
        rstd_of((xt, xt[:]), hh_, SSQ, RSTD, D)
        P.op("dve", lambda xt=xt, hh_=hh_: dve.scalar_tensor_tensor(
            out=hh_[:], in0=xt[:], scalar=RSTD[:, 0:1], in1=A1, op0=ALU.mult, op1=ALU.mult),
            reads=[xt, RSTD, MODB], writes=[hh_])
        P.op("dve", lambda hh_=hh_: dve.tensor_tensor(out=HB[:], in0=hh_[:], in1=SH1, op=ALU.add),
             reads=[hh_, MODB], writes=[HB])
        tb = nextbank()
        tbb = tb[:].bitcast(BF16)
        for kc in range(8):
            P.op("pe", lambda kc=kc, tbb=tbb: pe.transpose(tbb[:, kc * 128:(kc + 1) * 128], HB[:, kc * 128:(kc + 1) * 128], IDB[:]),
                 reads=[HB, IDB], writes=[tb], accum=(kc > 0))
        P.op("act", lambda tbb=tbb: act.copy(out=HT[:].rearrange("p a b -> p (a b)"), in_=tbb), reads=[tb], writes=[HT])

        def proj(n0, n):
            b = nextbank()
            for kc in range(8):
                P.op("pe", lambda kc=kc, b=b: pe.matmul(b[:, 0:n], lhsT=HT[:, kc, :], rhs=WIN[:, kc, n0:n0 + n],
                                                         start=(kc == 0), stop=(kc == 7)),
                     reads=[HT, WIN], writes=[b], accum=(kc > 0))
            return b
        b = proj(0, 512)
        P.op("dve", lambda b=b: dve.tensor_copy(out=QK[:, 0:512], in_=b[:]), reads=[b], writes=[QK])
        b = proj(512, 512)
        P.op("act", lambda b=b: act.copy(out=QK[:, 512:1024], in_=b[:]), reads=[b], writes=[QK], accum=True)
        b = proj(1024, 512)
        P.op("dve", lambda b=b: dve.tensor_copy(out=V[:, 0:512], in_=b[:]), reads=[b], writes=[V])
        b = proj(1536, 512)
        P.op("act", lambda b=b: act.copy(out=V[:, 512:1024], in_=b[:]), reads=[b], writes=[V], accum=True)
        b = proj(2048, 512)
        P.op("act", lambda b=b: act.activation(out=SR[:, 0:512], in_=b[:], func=ACT.Silu), reads=[b], writes=[SR])
        b = proj(2560, 512)
        P.op("act", lambda b=b: act.activation(out=SR[:, 512:1024], in_=b[:], func=ACT.Silu), reads=[b], writes=[SR], accum=True)
        b = proj(3072, 16)
        P.op("dve", lambda b=b: dve.tensor_copy(out=Z[:], in_=b[:, 0:16]), reads=[b], writes=[Z])
        P.op("dve", lambda: dve.tensor_tensor(out=SR[:].rearrange("p (h v) -> p h v", h=4), in0=SR[:].rearrange("p (h v) -> p h v", h=4),
                                              in1=GN[:].unsqueeze(1).broadcast_to([128, 4, 256]), op=ALU.mult),
             reads=[SR, GN], writes=[SR])
        if gstop < 2:
            continue
        b = nextbank()
        P.op("pe", lambda b=b: pe.transpose(b[0:16, 0:128], Z[:], ident), reads=[Z, CST], writes=[b])
        P.op("act", lambda b=b: act.copy(out=ZT[:], in_=b[0:16, 0:128]), reads=[b], writes=[ZT])
        b = nextbank()
        P.op("pe", lambda b=b: pe.matmul(b[:], lhsT=ZT[:], rhs=WG[:], start=True, stop=False), reads=[ZT, WG], writes=[b])
        P.op("pe", lambda b=b: pe.matmul(b[:], lhsT=ones[0:1, :], rhs=BG[0:1, :], start=False, stop=True),
             reads=[CST, BG], writes=[b], accum=True)
        P.op("act", lambda b=b: act.activation(out=G[:], in_=b[:], func=ACT.Exp, scale=-1.0), reads=[b], writes=[G])
        P.op("act", lambda: act.activation(out=G[:], in_=G[:], func=ACT.Ln, bias=1.0), reads=[G], writes=[G])
        b = nextbank()
        for h in range(4):
            P.op("pe", lambda h=h, b=b: pe.matmul(b[:, h * 128:(h + 1) * 128], lhsT=G[:, h * 128:(h + 1) * 128], rhs=C("tri2"),
                                                   start=True, stop=True), reads=[G, CST], writes=[b], accum=(h > 0))
        P.op("act", lambda b=b: act.activation(out=EB[:].rearrange("p a b -> p (a b)"), in_=b[:], func=ACT.Exp, scale=-GT),
             reads=[b], writes=[EB])
        P.op("act", lambda b=b: act.activation(out=ENB[:].rearrange("p a b -> p (a b)"), in_=b[:], func=ACT.Exp, scale=GT),
             reads=[b], writes=[ENB])
        b = nextbank()
        P.op("pe", lambda b=b: pe.matmul(b[:], lhsT=C("triu2"), rhs=G[:], start=True, stop=True), reads=[G, CST], writes=[b])
        P.op("act", lambda b=b: act.activation(out=EREV[:], in_=b[:], func=ACT.Exp, scale=-GT), reads=[b], writes=[EREV])
        if gstop < 3:
            continue
        qb, kb = nextbank(), nextbank()
        for h in range(4):
            P.op("pe", lambda h=h, qb=qb: pe.transpose(qb[:, h * 128:(h + 1) * 128], QK[:, h * 128:(h + 1) * 128], ident),
                 reads=[QK, CST], writes=[qb], accum=(h > 0))
        for h in range(4):
            P.op("pe", lambda h=h, kb=kb: pe.transpose(kb[:, h * 128:(h + 1) * 128], QK[:, 512 + h * 128:512 + (h + 1) * 128], ident),
                 reads=[QK, CST], writes=[kb], accum=(h > 0))
        qb3 = qb[:].rearrange("p (h t) -> p h t", h=4)
        for cc in range(2):
            P.op("dve", lambda cc=cc, qb3=qb3: dve.scalar_tensor_tensor(
                out=QEZ[:, :, cc, cc * 64:(cc + 1) * 64], in0=qb3[:, :, cc * 64:(cc + 1) * 64], scalar=SC,
                in1=EB[:, :, cc * 64:(cc + 1) * 64], op0=ALU.mult, op1=ALU.mult),
                reads=[qb, EB], writes=[QEZ], accum=(cc > 0))
        P.op("dve", lambda kb=kb: dve.tensor_tensor(out=KE[:].rearrange("p a b -> p (a b)"), in0=kb[:],
                                                    in1=ENB[:].rearrange("p a b -> p (a b)"), op=ALU.mult),
             reads=[kb, ENB], writes=[KE])
        P.op("dve", lambda: dve.tensor_tensor(out=KD0[0:64, :], in0=QK[0:64, 512:1024], in1=EREV[0:64, :], op=ALU.mult),
             reads=[QK, EREV], writes=[KD0])
        P.op("dve", lambda: dve.tensor_tensor(out=KD1[64:128, :], in0=QK[64:128, 512:1024], in1=EREV[64:128, :], op=ALU.mult),
             reads=[QK, EREV], writes=[KD1])
        if gstop < 4:
            continue
        PSBl = env["PSB"]
        ob = [PSBl[0], PSBl[1]]
        scb = PSBl[2]
        sub_ = [PSBl[3], PSBl[4]]
        su2b = [PSBl[5], PSBl[6]]
        S0 = SA if i % 2 == 0 else SN
        S2 = SN if i % 2 == 0 else SA
        for h in range(4):
            o_sc = scb[:, h * 128:(h + 1) * 128]
            P.op("pe", lambda h=h, o_sc=o_sc: pe.matmul(o_sc, lhsT=KE[:, h, :], rhs=QEZ[:, h, 0, :], start=True, stop=False),
                 reads=[KE, QEZ], writes=[scb], accum=(h > 0))
            P.op("pe", lambda h=h, o_sc=o_sc: pe.matmul(o_sc, lhsT=KE[:, h, :], rhs=QEZ[:, h, 1, :], start=False, stop=True),
                 reads=[KE, QEZ], writes=[scb], accum=True)
        for h in range(4):
            vh = V[:, h * 256:(h + 1) * 256]
            P.op("pe", lambda h=h, vh=vh: pe.matmul(sub_[h // 2][:, (h % 2) * 256:(h % 2 + 1) * 256],
                                                    lhsT=KD0[:, h * 128:(h + 1) * 128], rhs=vh, start=True, stop=True),
                 reads=[KD0, V], writes=[sub_[h // 2]], accum=(h % 2 > 0))
        for h in range(4):
            vh = V[:, h * 256:(h + 1) * 256]
            P.op("pe", lambda h=h, vh=vh: pe.matmul(su2b[h // 2][:, (h % 2) * 256:(h % 2 + 1) * 256],
                                                    lhsT=KD1[:, h * 128:(h + 1) * 128], rhs=vh, start=True, stop=True),
                 reads=[KD1, V], writes=[su2b[h // 2]], accum=(h % 2 > 0))
        P.op("dve", lambda: dve.tensor_tensor(out=ST4[:], in0=scb[:].rearrange("p (h t) -> p h t", h=4),
                                              in1=C("tri2").unsqueeze(1).broadcast_to([128, 4, 128]), op=ALU.mult),
             reads=[scb, CST], writes=[ST4])
        for h in range(4):
            P.op("dve", lambda h=h: dve.scalar_tensor_tensor(
                out=SB[h][:], in0=S0[h][:], scalar=EB[:, h, 63:64], in1=sub_[h // 2][:, (h % 2) * 256:(h % 2 + 1) * 256],
                op0=ALU.mult, op1=ALU.add), reads=[S0[h], EB, sub_[h // 2]], writes=[SB[h]])
        for h in range(4):
            P.op("dve", lambda h=h: dve.scalar_tensor_tensor(
                out=S2[h][:], in0=SB[h][:], scalar=EB[:, h, 127:128], in1=su2b[h // 2][:, (h % 2) * 256:(h % 2 + 1) * 256],
                op0=ALU.mult, op1=ALU.add), reads=[SB[h], EB, su2b[h // 2]], writes=[S2[h]])
        for h in range(4):
            vh = V[:, h * 256:(h + 1) * 256]
            o_ap = ob[h // 2][:, (h % 2) * 256:(h % 2 + 1) * 256]
            P.op("pe", lambda h=h, vh=vh, o_ap=o_ap: pe.matmul(o_ap, lhsT=ST4[:, h, :], rhs=vh, start=True, stop=False),
                 reads=[ST4, V], writes=[ob[h // 2]], accum=(h % 2 > 0))
            P.op("pe", lambda h=h, o_ap=o_ap: pe.matmul(o_ap, lhsT=QEZ[:, h, 0, :], rhs=S0[h][:], start=False, stop=False),
                 reads=[QEZ, S0[h]], writes=[ob[h // 2]], accum=True)
            P.op("pe", lambda h=h, o_ap=o_ap: pe.matmul(o_ap, lhsT=QEZ[:, h, 1, :], rhs=SB[h][:], start=False, stop=True),
                 reads=[QEZ, SB[h]], writes=[ob[h // 2]], accum=True)
        if gstop < 5:
            continue
        OS = QK
        P.op("act", lambda: act.copy(out=OS[:, 0:512], in_=ob[0][:]), reads=[ob[0]], writes=[OS])
        P.op("act", lambda: act.copy(out=OS[:, 512:1024], in_=ob[1][:]), reads=[ob[1]], writes=[OS], accum=True)
        for h in range(4):
            oh = OS[:, h * 256:(h + 1) * 256]
            P.op("dve", lambda h=h, oh=oh: dve.scalar_tensor_tensor(out=JK[:], in0=oh, scalar=1.0, in1=oh, op0=ALU.mult,
                                                                    op1=ALU.mult, accum_out=SS4[:, h:h + 1]),
                 reads=[OS], writes=[JK, SS4])
        P.op("act", lambda: act.activation(out=RS4[:], in_=SS4[:], func=ACT.Sqrt, scale=1.0 / 256, bias=EPS),
             reads=[SS4], writes=[RS4])
        P.op("dve", lambda: dve.reciprocal(out=RS4[:], in_=RS4[:]), reads=[RS4], writes=[RS4])
        for h in range(4):
            P.op("dve", lambda h=h: dve.scalar_tensor_tensor(
                out=V[:, h * 256:(h + 1) * 256], in0=OS[:, h * 256:(h + 1) * 256], scalar=RS4[:, h:h + 1],
                in1=SR[:, h * 256:(h + 1) * 256], op0=ALU.mult, op1=ALU.mult),
                reads=[OS, RS4, SR], writes=[V], accum=(h > 0))
        P.op("act", lambda: act.copy(out=HB[:], in_=V[:]), reads=[V], writes=[HB])
        if gstop < 6:
            continue
        tb = nextbank()
        tbb = tb[:].bitcast(BF16)
        for kc in range(8):
            P.op("pe", lambda kc=kc, tbb=tbb: pe.transpose(tbb[:, kc * 128:(kc + 1) * 128], HB[:, kc * 128:(kc + 1) * 128], IDB[:]),
                 reads=[HB, IDB], writes=[tb], accum=(kc > 0))
        P.op("act", lambda tbb=tbb: act.copy(out=HT[:].rearrange("p a b -> p (a b)"), in_=tbb), reads=[tb], writes=[HT])
        yb = [nextbank(), nextbank()]
        for q in range(2):
            for kc in range(8):
                P.op("pe", lambda q=q, kc=kc: pe.matmul(yb[q][:], lhsT=HT[:, kc, :], rhs=WOUT[:, kc, q * 512:(q + 1) * 512],
                                                        start=(kc == 0), stop=(kc == 7)),
                     reads=[HT, WOUT], writes=[yb[q]], accum=(kc > 0))
        if gstop < 7:
            continue
        yo = hh_
        for q in range(2):
            sl = slice(q * 512, (q + 1) * 512)
            P.op("dve", lambda q=q, sl=sl, yo=yo: dve.tensor_tensor(out=yo[:, sl], in0=yb[q][:], in1=G1[:, sl], op=ALU.mult),
                 reads=[yb[q], MODB], writes=[yo], accum=(q > 0))
        P.op("dve", lambda yo=yo, xt=xt: dve.tensor_tensor(out=yo[:], in0=yo[:], in1=xt[:], op=ALU.add),
             reads=[yo, xt], writes=[yo])
        P.dma("sp", lambda yo=yo, i=i: sp.dma_start(out=out[i * 128:(i + 1) * 128, :], in_=yo[:]),
              reads=[yo], writes=[XD[i]], sembuf=yo)


_CACHE = {}


def kernel(**inputs):
    nl = DEPTH
    if "nc" not in _CACHE:
        _CACHE["nc"] = build(nl)
    nc = _CACHE["nc"]
    f = lambda k: np.ascontiguousarray(np.asarray(inputs[k], dtype=np.float32))
    shared = {k: f(k) for k in inputs if k not in ("x", "c")}
    shared["final_norm_g"] = shared["final_norm_g"].reshape(1, D)
    shared["consts"] = CONST_ARR
    x = f("x")
    c = f("c")
    in_maps = []
    for b in range(8):
        m = dict(shared)
        m["x"] = x[b]
        m["c"] = c[b:b + 1]
        in_maps.append(m)
    res = run_bass_kernel_spmd(nc, in_maps, core_ids=list(range(8)))
    return np.stack([r["out"] for r in res.results], axis=0).astype(np.float32)
```

```python
import numpy as np
import concourse.bass as bass
import concourse.mybir as mybir
from concourse.bass_utils import run_bass_kernel_spmd

F32 = mybir.dt.float32
BF16 = mybir.dt.bfloat16
F32R = mybir.dt.float32r
I32 = mybir.dt.int32
ALU = mybir.AluOpType
ACT = mybir.ActivationFunctionType
AX = mybir.AxisListType

D = 1024
S = 4096
NT = S // 128
DEPTH = 4
EPS = 1e-6
NE = 64
DFF = 384
NSLOT_T = 96
SLOT_R = 256
GIN = 3088


class Buf:
    __slots__ = ("name", "t", "last_w", "readers", "dsem", "multi")

    def __init__(self, name, t, multi=False):
        self.name = name
        self.t = t
        self.last_w = {}
        self.readers = {}
        self.dsem = None
        self.multi = multi

    def __getitem__(self, idx):
        return self.t[idx]


class Prog:
    ENG = ("pe", "act", "dve", "pool", "sp")

    def __init__(self, nc):
        self.nc = nc
        self.eng = {"pe": nc.tensor, "act": nc.scalar, "dve": nc.vector,
                    "pool": nc.gpsimd, "sp": nc.sync}
        self.sems = {}
        self.cnt = {}
        self.seen = {e: {} for e in self.ENG}
        self._stack = []
        self._semcms = []
        self._scoped = []
        self.free_dsems = []
        self.nbuf = 0
        self.nops = 0
        for e in self.ENG:
            self._newsem("E_" + e)

    def _enter(self, cm):
        v = cm.__enter__()
        self._stack.append(cm)
        return v

    def _newsem(self, key):
        cm = self.nc.semaphore("s_" + key)
        s = cm.__enter__()
        self._semcms.append(cm)
        self.sems[key] = s
        self.cnt[key] = 0
        return s

    def sbuf(self, name, shape, dt=F32):
        self.nbuf += 1
        name = "%s_%d" % (name, self.nbuf)
        b = Buf(name, self._enter(self.nc.sbuf_tensor(name, list(shape), dt)))
        self._scoped.append((len(self._stack), b))
        return b

    def psum(self, name, shape, dt=F32):
        return Buf(name, self._enter(self.nc.psum_tensor(name, list(shape), dt)))

    def dram(self, name, shape, dt=F32, kind="Internal", multi=False):
        return Buf(name, self.nc.dram_tensor(name, list(shape), dt, kind=kind), multi)

    def mark(self):
        return len(self._stack)

    def release(self, mark):
        self.barrier()
        while len(self._stack) > mark:
            self._stack.pop().__exit__(None, None, None)
        while self._scoped and self._scoped[-1][0] > mark:
            _, b = self._scoped.pop()
            if b.dsem is not None:
                self.free_dsems.append(b.dsem)
                b.dsem = None

    def barrier(self):
        for e in self.ENG:
            seen = self.seen[e]
            for k, v in self.cnt.items():
                if v > 0 and seen.get(k, 0) < v:
                    seen[k] = v
                    self.eng[e].wait_ge(self.sems[k], v)

    def _deps(self, e, reads, writes, accum):
        need = {}
        for b in reads:
            for k, v in b.last_w.items():
                if need.get(k, 0) < v:
                    need[k] = v
        for b in writes:
            if not (accum or b.multi):
                for k, v in b.last_w.items():
                    if need.get(k, 0) < v:
                        need[k] = v
            for k, v in b.readers.items():
                if need.get(k, 0) < v:
                    need[k] = v
        seen = self.seen[e]
        eng = self.eng[e]
        for k, v in need.items():
            if seen.get(k, 0) < v:
                seen[k] = v
                eng.wait_ge(self.sems[k], v)

    def _mark(self, key, val, reads, writes, accum):
        for b in reads:
            b.readers[key] = val
        for b in writes:
            if accum or b.multi:
                b.last_w[key] = val
            else:
                b.last_w = {key: val}
                b.readers = {}

    def op(self, e, fn, reads=(), writes=(), accum=False):
        self._deps(e, reads, writes, accum)
        key = "E_" + e
        self.cnt[key] += 1
        fn().then_inc(self.sems[key], 1)
        self._mark(key, self.cnt[key], reads, writes, accum)
        self.nops += 1

    def dma(self, e, fn, reads=(), writes=(), sembuf=None, accum=False):
        self._deps(e, reads, writes, accum)
        sb = sembuf if sembuf is not None else (writes[0] if writes else reads[0])
        if sb.dsem is None:
            if self.free_dsems:
                sb.dsem = self.free_dsems.pop()
            else:
                self.nbuf += 1
                sb.dsem = "D%d" % self.nbuf
                self._newsem(sb.dsem)
        key = sb.dsem
        self.cnt[key] += 16
        fn().then_inc(self.sems[key], 16)
        self._mark(key, self.cnt[key], reads, writes, accum)
        self.nops += 1

    def wait_for(self, e, bufs):
        self._deps(e, bufs, (), False)

    def finish(self):
        while self._stack:
            self._stack.pop().__exit__(None, None, None)
        while self._semcms:
            self._semcms.pop().__exit__(None, None, None)


POOL_WINDOWS = (2, 4, 8, 16)


def _consts():
    c = {}
    t = np.arange(128)
    c["ident"] = np.eye(128, dtype=np.float32)
    c["ones"] = np.ones((128, 128), np.float32)
    same = (t[:, None] // 64) == (t[None, :] // 64)
    c["tri2"] = (same & (t[:, None] <= t[None, :])).astype(np.float32)
    c["triu2"] = (same & (t[:, None] > t[None, :])).astype(np.float32)
    c["u128"] = (t[:, None] < t[None, :]).astype(np.float32)
    c["pidx"] = np.tile(np.arange(128, dtype=np.float32)[:, None], (1, 128))
    c["jt2"] = np.tile((256.0 * np.arange(128, dtype=np.float32))[None, :], (128, 1))
    c["jt"] = np.tile((128.0 * np.arange(128, dtype=np.float32))[None, :], (128, 1))
    for w in POOL_WINDOWS:
        d = t[None, :] - t[:, None]
        cur = ((d >= 0) & (d < w)).astype(np.float32) / w - np.eye(128, dtype=np.float32)
        first = ((d >= 0) & (d < w)).astype(np.float32) / np.minimum(t + 1, w)[None, :].astype(np.float32) \
            - np.eye(128, dtype=np.float32)
        dp = t[None, :] + 128 - t[:, None]
        prev = ((dp >= 0) & (dp < w)).astype(np.float32) / w
        c["pwc%d" % w] = cur.astype(np.float32)
        c["pw0%d" % w] = first.astype(np.float32)
        c["pwp%d" % w] = prev.astype(np.float32)
    names = list(c.keys())
    arr = np.concatenate([c[n] for n in names], axis=1).astype(np.float32)
    offs = {n: i * 128 for i, n in enumerate(names)}
    return arr, offs


CONST_ARR, CONST_OFF = _consts()
NCONST = CONST_ARR.shape[1]


def build(nlayers=DEPTH, dbg=False, layers=None, nomoe=False, gstop=99):
    nc = bass.Bass("TRN2", target_bir_lowering=False)
    P = Prog(nc)
    pe, act, dve, pool, sp = nc.tensor, nc.scalar, nc.vector, nc.gpsimd, nc.sync

    def din(name, shape, dt=F32):
        return P.dram(name, shape, dt, kind="ExternalInput")

    x_in = din("x", [S, D])
    c_in = din("c", [1, D])
    norm_gain = din("norm_gain", [DEPTH, 2, D])
    w_mod = din("w_mod", [DEPTH, D, 6 * D])
    b_mod = din("b_mod", [DEPTH, 6 * D])
    pool_w = din("pool_w", [2, 4, 256, 256])
    pool_b = din("pool_b", [2, D])
    pool_scale = din("pool_scale", [2, D])
    gla_w_in = din("gla_w_in", [2, D, GIN])
    gla_w_gate = din("gla_w_gate", [2, 16, 512])
    gla_b_gate = din("gla_b_gate", [2, 512])
    gla_norm_g = din("gla_norm_g", [2, 256])
    gla_w_out = din("gla_w_out", [2, D, D])
    moe_w_group = din("moe_w_group", [DEPTH, D, 8])
    moe_b_group = din("moe_b_group", [DEPTH, 8])
    moe_w_expert = din("moe_w_expert", [DEPTH, D, NE])
    moe_b_expert = din("moe_b_expert", [DEPTH, NE])
    moe_w_in = din("moe_w_in", [DEPTH, NE, D, 2 * DFF])
    moe_w_out = din("moe_w_out", [DEPTH, NE, DFF, D])
    final_norm_g = din("final_norm_g", [1, D])
    consts_in = din("consts", [128, NCONST])

    out = P.dram("out", [S, D], F32, kind="ExternalOutput")
    h2d = P.dram("h2d", [S, D], F32)
    xs_d = P.dram("xs_d", [NSLOT_T * SLOT_R, D], F32, multi=True)
    ys_d = P.dram("ys_d", [NSLOT_T * SLOT_R, D], F32, multi=True)
    XD = [Buf("xd%d" % i, out.t) for i in range(NT)]
    HD = [Buf("hd%d" % i, h2d.t) for i in range(NT)]

    CST = P.sbuf("cst", [128, NCONST])
    MODB = P.sbuf("modb", [128, 6 * D])
    PSB = [P.psum("psb%d" % i, [128, 512]) for i in range(8)]
    P.dma("sp", lambda: sp.dma_start(out=CST[:], in_=consts_in[:]), writes=[CST])

    def C(name):
        o = CONST_OFF[name]
        return CST[:, o:o + 128]

    ident = C("ident")
    ones = C("ones")
    SH1, A1, G1, SH2, A2, G2 = [MODB[:, k * D:(k + 1) * D] for k in range(6)]

    BREG = pool.to_reg(NSLOT_T * SLOT_R - 1)
    BREG2 = pool.to_reg(DEPTH * NE * 128 * 2 - 1)
    BREG3 = pool.to_reg(DEPTH * NE * 128 * 3 - 1)
    bank = [0]

    def nextbank():
        b = PSB[bank[0] % 8]
        bank[0] += 1
        return b

    def bcast_load(dst_buf, dst_ap, row_ap, n):
        P.dma("sp", lambda: sp.dma_start(out=dst_ap, in_=row_ap.partition_broadcast(128)), writes=[dst_buf])

    def rstd_of(xt, junk, ssq, rstd, n, eps=EPS):
        xb, xa = xt
        P.op("dve", lambda: dve.scalar_tensor_tensor(out=junk[:, 0:n], in0=xa, scalar=1.0, in1=xa, op0=ALU.mult,
                                                     op1=ALU.mult, accum_out=ssq[:, 0:1]),
             reads=[xb], writes=[junk, ssq])
        P.op("act", lambda: act.activation(out=rstd[:, 0:1], in_=ssq[:, 0:1], func=ACT.Sqrt, scale=1.0 / n, bias=eps),
             reads=[ssq], writes=[rstd])
        P.op("dve", lambda: dve.reciprocal(out=rstd[:, 0:1], in_=rstd[:, 0:1]), reads=[rstd], writes=[rstd])

    def xsrc(layer, i):
        if first[0]:
            return x_in, x_in[i * 128:(i + 1) * 128, :]
        return XD[i], out[i * 128:(i + 1) * 128, :]

    first = [True]
    for L in (layers if layers is not None else range(nlayers)):
        j2 = L // 2
        is_pool = (L % 2 == 0)
        m0 = P.mark()
        CT = P.sbuf("ct", [128, 8])
        SCB = P.sbuf("scb", [128, 8, 128])
        WM = [P.sbuf("wm%d" % i, [128, 3072]) for i in range(2)]
        BROW = P.sbuf("brow", [1, 6 * D])
        NGB = P.sbuf("ngb", [128, 2 * D])
        with nc.allow_non_contiguous_dma(reason="tiny c column load"):
            P.dma("sp", lambda: sp.dma_start(out=CT[:], in_=c_in[0:1, :].rearrange("a (k p) -> p (a k)", p=128)),
                  writes=[CT])
        P.op("act", lambda: act.activation(out=CT[:], in_=CT[:], func=ACT.Silu), reads=[CT], writes=[CT])
        for kc in range(8):
            P.op("dve", lambda kc=kc: dve.tensor_scalar(out=SCB[:, kc, :], in0=ones, scalar1=CT[:, kc:kc + 1],
                                                        scalar2=None, op0=ALU.mult),
                 reads=[CT, CST], writes=[SCB], accum=True)
        P.dma("sp", lambda: sp.dma_start(out=BROW[:], in_=b_mod[L:L + 1, :]), writes=[BROW])
        bcast_load(NGB, NGB[:, 0:D], norm_gain[L, 0:1, :], D)
        P.dma("sp", lambda: sp.dma_start(out=NGB[:, D:2 * D], in_=norm_gain[L, 1:2, :].partition_broadcast(128)),
              writes=[NGB], accum=True)
        for half in range(2):
            banks = [nextbank() for _ in range(6)]
            for kc in range(8):
                wb = WM[kc % 2]
                P.dma("sp", lambda wb=wb, kc=kc, half=half: sp.dma_start(
                    out=wb[:], in_=w_mod[L, kc * 128:(kc + 1) * 128, half * 3072:(half + 1) * 3072]), writes=[wb])
                for n in range(6):
                    P.op("pe", lambda wb=wb, kc=kc, n=n, b=banks[n]: pe.matmul(
                        b[:], lhsT=SCB[:, kc, :], rhs=wb[:, n * 512:(n + 1) * 512], start=(kc == 0), stop=False),
                        reads=[SCB, wb], writes=[banks[n]], accum=(kc > 0))
            for n in range(6):
                col = half * 3072 + n * 512
                P.op("pe", lambda n=n, b=banks[n], col=col: pe.matmul(
                    b[:], lhsT=ones[0:1, :], rhs=BROW[0:1, col:col + 512], start=False, stop=True),
                    reads=[CST, BROW], writes=[banks[n]], accum=True)
                P.op("act", lambda b=banks[n], col=col: act.copy(out=MODB[:, col:col + 512], in_=b[:]),
                     reads=[banks[n]], writes=[MODB], accum=True)
        P.op("dve", lambda: dve.scalar_tensor_tensor(out=A1, in0=A1, scalar=1.0, in1=NGB[:, 0:D], op0=ALU.add,
                                                     op1=ALU.mult), reads=[MODB, NGB], writes=[MODB])
        P.op("dve", lambda: dve.scalar_tensor_tensor(out=A2, in0=A2, scalar=1.0, in1=NGB[:, D:2 * D], op0=ALU.add,
                                                     op1=ALU.mult), reads=[MODB, NGB], writes=[MODB])
        P.release(m0)

        m0 = P.mark()
        if is_pool:
            PWT = P.sbuf("pwt", [128, 4, 2, 256])
            SGB = P.sbuf("sgb", [128, D])
            BSG = P.sbuf("bsg", [128, D])
            XT = [P.sbuf("xt%d" % i, [128, D]) for i in range(2)]
            HN = [P.sbuf("hn%d" % i, [128, D]) for i in range(3)]
            JK = P.sbuf("jk", [128, D])
            SSQ = P.sbuf("ssq", [128, 1])
            RSTD = P.sbuf("rstd", [128, 1])
            DT = P.sbuf("dt", [128, 8, 128])
            YO = [P.sbuf("yo%d" % i, [128, D]) for i in range(2)]
            P.dma("sp", lambda: sp.dma_start(out=PWT[:], in_=pool_w[j2].rearrange("g (k p) n -> p g k n", p=128)),
                  writes=[PWT])
            bcast_load(SGB, SGB[:], pool_scale[j2:j2 + 1, :], D)
            bcast_load(BSG, BSG[:], pool_b[j2:j2 + 1, :], D)
            P.op("dve", lambda: dve.tensor_tensor(out=SGB[:], in0=SGB[:], in1=G1, op=ALU.mult),
                 reads=[SGB, MODB], writes=[SGB])
            P.op("dve", lambda: dve.tensor_tensor(out=BSG[:], in0=BSG[:], in1=SGB[:], op=ALU.mult),
                 reads=[BSG, SGB], writes=[BSG])
            def pool_front(i):
                xt, hn = XT[i % 2], HN[i % 3]
                sb, sa = xsrc(L, i)
                P.dma("act", lambda xt=xt, sa=sa: act.dma_start(out=xt[:], in_=sa), reads=[sb], writes=[xt])
                rstd_of((xt, xt[:]), JK, SSQ, RSTD, D)
                P.op("dve", lambda xt=xt, hn=hn: dve.scalar_tensor_tensor(
                    out=hn[:], in0=xt[:], scalar=RSTD[:, 0:1], in1=A1, op0=ALU.mult, op1=ALU.mult),
                    reads=[xt, RSTD, MODB], writes=[hn])

            pool_front(0)
            for i in range(NT):
                xt, hn, hp, yo = XT[i % 2], HN[i % 3], HN[(i - 1) % 3], YO[i % 2]
                pb = [nextbank(), nextbank()]
                for kc in range(8):
                    w = POOL_WINDOWS[kc // 2]
                    b = pb[kc // 4]
                    o_ap = b[:, (kc % 4) * 128:(kc % 4 + 1) * 128]
                    cur = C(("pw0%d" if i == 0 else "pwc%d") % w)
                    P.op("pe", lambda hn=hn, kc=kc, o_ap=o_ap, cur=cur, i=i: pe.matmul(
                        o_ap, lhsT=hn[:, kc * 128:(kc + 1) * 128], rhs=cur, start=True, stop=(i == 0)),
                        reads=[hn, CST], writes=[b], accum=(kc % 4 > 0))
                    if i > 0:
                        P.op("pe", lambda hp=hp, kc=kc, o_ap=o_ap, w=w: pe.matmul(
                            o_ap, lhsT=hp[:, kc * 128:(kc + 1) * 128], rhs=C("pwp%d" % w), start=False, stop=True),
                            reads=[hp, CST], writes=[b], accum=True)
                for hh in range(2):
                    P.op("act", lambda hh=hh, b=pb[hh]: act.copy(
                        out=DT[:, hh * 4:(hh + 1) * 4, :].rearrange("p a b -> p (a b)"), in_=b[:]),
                        reads=[pb[hh]], writes=[DT], accum=(hh > 0))
                if i + 1 < NT:
                    pool_front(i + 1)
                yb = [nextbank(), nextbank()]
                for g in range(4):
                    b = yb[g // 2]
                    o_ap = b[:, (g % 2) * 256:(g % 2 + 1) * 256]
                    for k in range(2):
                        P.op("pe", lambda g=g, k=k, o_ap=o_ap: pe.matmul(
                            o_ap, lhsT=DT[:, 2 * g + k, :], rhs=PWT[:, g, k, :], start=(k == 0), stop=(k == 1)),
                            reads=[DT, PWT], writes=[b], accum=(g % 2 > 0 or k > 0))
                for hh in range(2):
                    sl = slice(hh * 512, (hh + 1) * 512)
                    P.op("dve", lambda hh=hh, sl=sl, yo=yo, b=yb[hh]: dve.tensor_tensor(
                        out=yo[:, sl], in0=b[:], in1=SGB[:, sl], op=ALU.mult),
                        reads=[yb[hh], SGB], writes=[yo], accum=(hh > 0))
                P.op("dve", lambda yo=yo: dve.tensor_tensor(out=yo[:], in0=yo[:], in1=BSG[:], op=ALU.add),
                     reads=[yo, BSG], writes=[yo])
                P.op("dve", lambda yo=yo, xt=xt: dve.tensor_tensor(out=yo[:], in0=yo[:], in1=xt[:], op=ALU.add),
                     reads=[yo, xt], writes=[yo])
                P.dma("sp", lambda yo=yo, i=i: sp.dma_start(out=out[i * 128:(i + 1) * 128, :], in_=yo[:]),
                      reads=[yo], writes=[XD[i]], sembuf=yo)
        else:
            gla_layer(P, nc, L, j2, locals(), gstop)
        P.release(m0)
        first[0] = False

        if not nomoe:
            moe_layer(P, nc, L, locals())

    m0 = P.mark()
    FG = P.sbuf("fg", [128, D])
    XT = [P.sbuf("fxt%d" % i, [128, D]) for i in range(2)]
    YO = [P.sbuf("fyo%d" % i, [128, D]) for i in range(2)]
    JK = P.sbuf("fjk", [128, D])
    SSQ = P.sbuf("fssq", [128, 1])
    RSTD = P.sbuf("frstd", [128, 1])
    bcast_load(FG, FG[:], final_norm_g[0:1, :], D)
    for i in range(NT):
        xt, yo = XT[i % 2], YO[i % 2]
        sb, sa = xsrc(0, i)
        P.dma("act", lambda xt=xt, sa=sa: act.dma_start(out=xt[:], in_=sa), reads=[sb], writes=[xt])
        rstd_of((xt, xt[:]), JK, SSQ, RSTD, D)
        P.op("dve", lambda xt=xt, yo=yo: dve.scalar_tensor_tensor(
            out=yo[:], in0=xt[:], scalar=RSTD[:, 0:1], in1=FG[:], op0=ALU.mult, op1=ALU.mult),
            reads=[xt, RSTD, FG], writes=[yo])
        P.dma("sp", lambda yo=yo, i=i: sp.dma_start(out=out[i * 128:(i + 1) * 128, :], in_=yo[:]),
              reads=[yo], writes=[XD[i]], sembuf=yo)
    P.wait_for("sp", XD)
    P.release(m0)
    P.finish()
    return nc


def moe_layer(P, nc, L, env):
    pe, act, dve, pool, sp = nc.tensor, nc.scalar, nc.vector, nc.gpsimd, nc.sync
    C, ident, ones, nextbank, xsrc, rstd_of = [env[k] for k in ("C", "ident", "ones", "nextbank", "xsrc", "rstd_of")]
    A2, SH2, G2, MODB, CST = [env[k] for k in ("A2", "SH2", "G2", "MODB", "CST")]
    out, h2d, xs_d, ys_d, XD, HD = [env[k] for k in ("out", "h2d", "xs_d", "ys_d", "XD", "HD")]
    moe_w_group, moe_b_group, moe_w_expert, moe_b_expert, moe_w_in, moe_w_out = [
        env[k] for k in ("moe_w_group", "moe_b_group", "moe_w_expert", "moe_b_expert", "moe_w_in", "moe_w_out")]
    NS = NSLOT_T * SLOT_R
    BREG = env["BREG"]
    BREG2 = env["BREG2"]
    BREG3 = env["BREG3"]
    w_in_rows = moe_w_in.t.rearrange("l e (p k) n -> (l e p) (k n)", k=8).rearrange("r (m q) -> (r m) q", m=3)
    w_out_rows = moe_w_out.t.rearrange("l e (p k) n -> (l e p) (k n)", k=3).rearrange("r (m q) -> (r m) q", m=2)

    mR = P.mark()
    SL0 = P.sbuf("sl0", [128, NT], I32)
    SL1 = P.sbuf("sl1", [128, NT], I32)
    W0 = P.sbuf("w0", [128, NT])
    W1 = P.sbuf("w1", [128, NT])
    EJ = P.sbuf("ej", [128, NSLOT_T], I32)
    EJ3 = P.sbuf("ej3", [128, 3, NSLOT_T], I32)
    EJ2 = P.sbuf("ej2", [128, 2, NSLOT_T], I32)

    mA = P.mark()
    WR = P.sbuf("wr", [128, 8, 72])
    BR = P.sbuf("br", [1, 72])
    LG = P.sbuf("lg", [128, NT, 72])
    XT = [P.sbuf("mxt%d" % i, [128, D]) for i in range(2)]
    H2 = [P.sbuf("mh2%d" % i, [128, D]) for i in range(2)]
    H2T = P.sbuf("mh2t", [128, 8, 128])
    JK = P.sbuf("mjk", [128, D])
    SSQ = P.sbuf("mssq", [128, 1])
    RSTD = P.sbuf("mrstd", [128, 1])
    with nc.allow_non_contiguous_dma(reason="small router weight load"):
        P.dma("sp", lambda: sp.dma_start(out=WR[:, :, 0:8], in_=moe_w_group[L].rearrange("(k p) n -> p k n", p=128)),
              writes=[WR])
        P.dma("sp", lambda: sp.dma_start(out=WR[:, :, 8:72], in_=moe_w_expert[L].rearrange("(k p) n -> p k n", p=128)),
              writes=[WR], accum=True)
    P.dma("sp", lambda: sp.dma_start(out=BR[0:1, 0:8], in_=moe_b_group[L:L + 1, :]), writes=[BR])
    P.dma("sp", lambda: sp.dma_start(out=BR[0:1, 8:72], in_=moe_b_expert[L:L + 1, :]), writes=[BR], accum=True)
    def a1_front(i):
        xt, h2 = XT[i % 2], H2[i % 2]
        P.dma("act", lambda xt=xt, i=i: act.dma_start(out=xt[:], in_=out[i * 128:(i + 1) * 128, :]),
              reads=[XD[i]], writes=[xt])
        rstd_of((xt, xt[:]), JK, SSQ, RSTD, D)
        P.op("dve", lambda xt=xt, h2=h2: dve.scalar_tensor_tensor(
            out=h2[:], in0=xt[:], scalar=RSTD[:, 0:1], in1=A2, op0=ALU.mult, op1=ALU.mult),
            reads=[xt, RSTD, MODB], writes=[h2])
        P.op("dve", lambda h2=h2: dve.tensor_tensor(out=h2[:], in0=h2[:], in1=SH2, op=ALU.add),
             reads=[h2, MODB], writes=[h2])
        P.dma("sp", lambda h2=h2, i=i: sp.dma_start(out=h2d[i * 128:(i + 1) * 128, :], in_=h2[:]),
              reads=[h2], writes=[HD[i]], sembuf=h2)

    a1_front(0)
    for i in range(NT):
        xt, h2 = XT[i % 2], H2[i % 2]
        tb = [nextbank(), nextbank()]
        for kc in range(8):
            b = tb[kc // 4]
            P.op("pe", lambda h2=h2, kc=kc, b=b: pe.transpose(
                b[:, (kc % 4) * 128:(kc % 4 + 1) * 128], h2[:, kc * 128:(kc + 1) * 128], ident),
                reads=[h2, CST], writes=[b], accum=(kc % 4 > 0))
        for hh in range(2):
            P.op("act", lambda hh=hh, b=tb[hh]: act.copy(
                out=H2T[:, hh * 4:(hh + 1) * 4, :].rearrange("p a b -> p (a b)"), in_=b[:]),
                reads=[tb[hh]], writes=[H2T], accum=(hh > 0))
        if i + 1 < NT:
            a1_front(i + 1)
        lb = nextbank()
        for kc in range(8):
            P.op("pe", lambda kc=kc, lb=lb: pe.matmul(lb[:, 0:72], lhsT=H2T[:, kc, :], rhs=WR[:, kc, :],
                                                      start=(kc == 0), stop=False),
                 reads=[H2T, WR], writes=[lb], accum=(kc > 0))
        P.op("pe", lambda lb=lb: pe.matmul(lb[:, 0:72], lhsT=ones[0:1, :], rhs=BR[0:1, :], start=False, stop=True),
             reads=[CST, BR], writes=[lb], accum=True)
        P.op("act", lambda lb=lb, i=i: act.copy(out=LG[:, i, :], in_=lb[:, 0:72]), reads=[lb], writes=[LG], accum=True)

    def T3(name, n, dt=F32):
        return P.sbuf(name, [128, NT, n], dt)

    MG = P.sbuf("r_mg", [128, NT])
    MGM = T3("r_mgm", 8)
    EG = T3("r_eg", 8)
    PG = P.sbuf("r_pg", [128, NT])
    TMP = T3("r_tmp", 64)
    LS = T3("r_ls", 8)
    M1 = P.sbuf("r_m1", [128, NT])
    MK1 = T3("r_mk1", 8)
    LS2 = T3("r_ls2", 8)
    M2 = P.sbuf("r_m2", [128, NT])
    MK2 = T3("r_mk2", 8)
    E2 = P.sbuf("r_e2", [128, NT])
    DEN = P.sbuf("r_den", [128, NT])
    M1F = T3("r_m1f", 64)
    M2F = T3("r_m2f", 64)
    SEL = T3("r_sel", 64)
    CS = T3("r_cs", 64)
    POS = T3("r_pos", 64)
    CNT = P.sbuf("r_cnt", [128, 64])
    PC = P.sbuf("r_pc", [128, 64])
    INC = P.sbuf("r_inc", [128, 64])
    OFF = P.sbuf("r_off", [128, 64])
    SLF = P.sbuf("r_slf", [128, NT])
    EJF = P.sbuf("r_ejf", [128, NSLOT_T])
    TMP2 = P.sbuf("r_tmp2", [128, 64, 32])

    def bc(ap, shape):
        return ap.broadcast_to(shape)

    lg = LG[:, :, 0:8]
    P.op("dve", lambda: dve.tensor_reduce(out=MG[:], in_=lg, axis=AX.X, op=ALU.max), reads=[LG], writes=[MG])
    P.op("dve", lambda: dve.tensor_tensor(out=MGM[:], in0=lg, in1=bc(MG[:].unsqueeze(2), [128, NT, 8]),
                                          op=ALU.is_equal), reads=[LG, MG], writes=[MGM])
    P.op("dve", lambda: dve.tensor_tensor(out=EG[:], in0=lg, in1=bc(MG[:].unsqueeze(2), [128, NT, 8]),
                                          op=ALU.subtract), reads=[LG, MG], writes=[EG])
    P.op("act", lambda: act.activation(out=EG[:], in_=EG[:], func=ACT.Exp), reads=[EG], writes=[EG])
    P.op("dve", lambda: dve.tensor_reduce(out=PG[:], in_=EG[:], axis=AX.X, op=ALU.add), reads=[EG], writes=[PG])
    P.op("dve", lambda: dve.reciprocal(out=PG[:], in_=PG[:]), reads=[PG], writes=[PG])
    le4 = LG[:, :, 8:72].rearrange("p t (g e) -> p t g e", g=8)
    P.op("dve", lambda: dve.tensor_tensor(out=TMP[:].rearrange("p t (g e) -> p t g e", g=8), in0=le4,
                                          in1=bc(MGM[:].unsqueeze(3), [128, NT, 8, 8]), op=ALU.mult),
         reads=[LG, MGM], writes=[TMP])
    P.op("dve", lambda: dve.tensor_reduce(out=LS[:], in_=TMP[:].rearrange("p t (g e) -> p t e g", g=8), axis=AX.X,
                                          op=ALU.add), reads=[TMP], writes=[LS])
    P.op("dve", lambda: dve.tensor_reduce(out=M1[:], in_=LS[:], axis=AX.X, op=ALU.max), reads=[LS], writes=[M1])
    P.op("dve", lambda: dve.tensor_tensor(out=MK1[:], in0=LS[:], in1=bc(M1[:].unsqueeze(2), [128, NT, 8]),
                                          op=ALU.is_equal), reads=[LS, M1], writes=[MK1])
    P.op("dve", lambda: dve.scalar_tensor_tensor(out=LS2[:], in0=MK1[:], scalar=-1e30, in1=LS[:], op0=ALU.mult,
                                                 op1=ALU.add), reads=[MK1, LS], writes=[LS2])
    P.op("dve", lambda: dve.tensor_reduce(out=M2[:], in_=LS2[:], axis=AX.X, op=ALU.max), reads=[LS2], writes=[M2])
    P.op("dve", lambda: dve.tensor_tensor(out=MK2[:], in0=LS2[:], in1=bc(M2[:].unsqueeze(2), [128, NT, 8]),
                                          op=ALU.is_equal), reads=[LS2, M2], writes=[MK2])
    P.op("dve", lambda: dve.tensor_tensor(out=E2[:], in0=M2[:], in1=M1[:], op=ALU.subtract), reads=[M1, M2], writes=[E2])
    P.op("act", lambda: act.activation(out=E2[:], in_=E2[:], func=ACT.Exp), reads=[E2], writes=[E2])
    P.op("dve", lambda: dve.tensor_scalar(out=DEN[:], in0=E2[:], scalar1=1.0, scalar2=None, op0=ALU.add),
         reads=[E2], writes=[DEN])
    P.op("dve", lambda: dve.reciprocal(out=DEN[:], in_=DEN[:]), reads=[DEN], writes=[DEN])
    P.op("dve", lambda: dve.tensor_tensor(out=W0[:], in0=PG[:], in1=DEN[:], op=ALU.mult), reads=[PG, DEN], writes=[W0])
    P.op("dve", lambda: dve.tensor_tensor(out=W1[:], in0=W0[:], in1=E2[:], op=ALU.mult), reads=[W0, E2], writes=[W1])
    for MF, MK in ((M1F, MK1), (M2F, MK2)):
        P.op("dve", lambda MF=MF, MK=MK: dve.tensor_tensor(
            out=MF[:].rearrange("p t (g e) -> p t g e", g=8), in0=bc(MGM[:].unsqueeze(3), [128, NT, 8, 8]),
            in1=bc(MK[:].unsqueeze(2), [128, NT, 8, 8]), op=ALU.mult), reads=[MGM, MK], writes=[MF])
    P.op("dve", lambda: dve.tensor_tensor(out=SEL[:], in0=M1F[:], in1=M2F[:], op=ALU.add), reads=[M1F, M2F], writes=[SEL])
    P.op("dve", lambda: dve.memset(CS[:, 0, :], 0.0), writes=[CS])
    for i in range(1, NT):
        P.op("dve", lambda i=i: dve.tensor_tensor(out=CS[:, i, :], in0=CS[:, i - 1, :], in1=SEL[:, i - 1, :], op=ALU.add),
             reads=[CS, SEL], writes=[CS])
    P.op("dve", lambda: dve.tensor_tensor(out=CNT[:], in0=CS[:, NT - 1, :], in1=SEL[:, NT - 1, :], op=ALU.add),
         reads=[CS, SEL], writes=[CNT])
    for q in range(NT // 8):
        b = nextbank()
        for ii in range(8):
            i = q * 8 + ii
            o_ap = b[:, ii * 64:(ii + 1) * 64]
            P.op("pe", lambda i=i, o_ap=o_ap: pe.matmul(o_ap, lhsT=C("u128"), rhs=SEL[:, i, :], start=True, stop=False),
                 reads=[CST, SEL], writes=[b], accum=(ii > 0))
            P.op("pe", lambda i=i, o_ap=o_ap: pe.matmul(o_ap, lhsT=ones, rhs=CS[:, i, :], start=False, stop=True),
                 reads=[CST, CS], writes=[b], accum=True)
        P.op("act", lambda q=q, b=b: act.copy(out=POS[:, q * 8:(q + 1) * 8, :].rearrange("p a b -> p (a b)"), in_=b[:]),
             reads=[b], writes=[POS], accum=(q > 0))
    b = nextbank()
    P.op("pe", lambda: pe.matmul(b[:, 0:64], lhsT=ones, rhs=CNT[:], start=True, stop=True), reads=[CST, CNT], writes=[b])
    P.op("act", lambda: act.copy(out=CNT[:], in_=b[:, 0:64]), reads=[b], writes=[CNT])
    P.op("dve", lambda: dve.tensor_tensor(out=TMP2[:], in0=bc(CNT[:].unsqueeze(2), [128, 64, 32]),
        in1=bc(C("jt2")[:, 0:32].unsqueeze(1), [128, 64, 32]), op=ALU.is_gt), reads=[CNT, CST], writes=[TMP2])
    P.op("dve", lambda: dve.tensor_reduce(out=PC[:], in_=TMP2[:], axis=AX.X, op=ALU.add), reads=[TMP2], writes=[PC])
    P.op("dve", lambda: dve.tensor_scalar(out=PC[:], in0=PC[:], scalar1=float(SLOT_R), scalar2=None, op0=ALU.mult),
         reads=[PC], writes=[PC])
    P.op("dve", lambda: dve.tensor_tensor_scan(out=INC[:], data0=ones[:, 0:64], data1=PC[:], initial=0.0,
                                               op0=ALU.mult, op1=ALU.add), reads=[CST, PC], writes=[INC])
    P.op("dve", lambda: dve.tensor_tensor(out=OFF[:], in0=INC[:], in1=PC[:], op=ALU.subtract), reads=[INC, PC], writes=[OFF])
    P.op("dve", lambda: dve.tensor_tensor(out=POS[:], in0=POS[:], in1=bc(OFF[:].unsqueeze(1), [128, NT, 64]), op=ALU.add),
         reads=[POS, OFF], writes=[POS])
    for MF, SL in ((M1F, SL0), (M2F, SL1)):
        P.op("dve", lambda MF=MF: dve.tensor_tensor(out=TMP[:], in0=MF[:], in1=POS[:], op=ALU.mult),
             reads=[MF, POS], writes=[TMP])
        P.op("dve", lambda: dve.tensor_reduce(out=SLF[:], in_=TMP[:], axis=AX.X, op=ALU.add), reads=[TMP], writes=[SLF])
        P.op("dve", lambda SL=SL: dve.tensor_copy(out=SL[:], in_=SLF[:]), reads=[SLF], writes=[SL])
    for q in range(NSLOT_T // 32):
        P.op("dve", lambda q=q: dve.tensor_tensor(
            out=TMP[:], in0=bc(INC[:].unsqueeze(1), [128, 32, 64]),
            in1=bc(C("jt2")[:, q * 32:(q + 1) * 32].unsqueeze(2), [128, 32, 64]), op=ALU.is_le),
            reads=[INC, CST], writes=[TMP])
        P.op("dve", lambda q=q: dve.tensor_reduce(out=EJF[:, q * 32:(q + 1) * 32], in_=TMP[:], axis=AX.X, op=ALU.add),
             reads=[TMP], writes=[EJF], accum=(q > 0))
    SKP = P.sbuf("r_skp", [128, NSLOT_T])
    P.op("dve", lambda: dve.tensor_scalar(out=SKP[:], in0=EJF[:], scalar1=63.5, scalar2=1.0e6, op0=ALU.is_gt, op1=ALU.mult),
         reads=[EJF], writes=[SKP])
    P.op("dve", lambda: dve.tensor_tensor(out=EJF[:], in0=EJF[:], in1=SKP[:], op=ALU.add), reads=[EJF, SKP], writes=[EJF])
    P.op("dve", lambda: dve.tensor_scalar(out=EJF[:], in0=EJF[:], scalar1=128.0, scalar2=float(L * NE * 128),
                                          op0=ALU.mult, op1=ALU.add), reads=[EJF], writes=[EJF])
    P.op("dve", lambda: dve.tensor_tensor(out=EJF[:], in0=EJF[:], in1=C("pidx")[:, 0:NSLOT_T], op=ALU.add),
         reads=[EJF, CST], writes=[EJF])
    P.op("dve", lambda: dve.tensor_copy(out=EJ[:], in_=EJF[:]), reads=[EJF], writes=[EJ])
    EJG = P.sbuf("r_ejg", [128, NSLOT_T])
    for m in range(3):
        P.op("dve", lambda m=m: dve.tensor_scalar(out=EJG[:], in0=EJF[:], scalar1=3.0, scalar2=float(m), op0=ALU.mult,
                                                  op1=ALU.add), reads=[EJF], writes=[EJG])
        P.op("dve", lambda m=m: dve.tensor_copy(out=EJ3[:, m, :], in_=EJG[:]), reads=[EJG], writes=[EJ3], accum=(m > 0))
    for m in range(2):
        P.op("dve", lambda m=m: dve.tensor_scalar(out=EJG[:], in0=EJF[:], scalar1=2.0, scalar2=float(m), op0=ALU.mult,
                                                  op1=ALU.add), reads=[EJF], writes=[EJG])
        P.op("dve", lambda m=m: dve.tensor_copy(out=EJ2[:, m, :], in_=EJG[:]), reads=[EJG], writes=[EJ2], accum=(m > 0))

    for i in range(NT):
        h2 = H2[i % 2]
        P.dma("sp", lambda h2=h2, i=i: sp.dma_start(out=h2[:], in_=h2d[i * 128:(i + 1) * 128, :]),
              reads=[HD[i]], writes=[h2])
        for SL in (SL0, SL1):
            P.dma("pool", lambda h2=h2, SL=SL, i=i: pool.indirect_dma_start(
                out=xs_d[:, :], out_offset=bass.IndirectOffsetOnAxis(ap=SL[:, i:i + 1], axis=0), in_=h2[:, :],
                in_offset=None, bounds_check=BREG, oob_is_err=False),
                reads=[h2, SL], writes=[xs_d], sembuf=h2)
    P.release(mA)

    mB = P.mark()
    WI = [P.sbuf("wi%d" % i, [128, 8, 2 * DFF]) for i in range(3)]
    WO = [P.sbuf("wo%d" % i, [128, 3, D]) for i in range(3)]
    XS = [P.sbuf("xs%d" % i, [128, D]) for i in range(3)]
    XT8 = [P.sbuf("xt8%d" % i, [128, 8, 128]) for i in range(2)]
    SG = P.sbuf("sg", [128, DFF])
    HS = [P.sbuf("hs%d" % i, [128, DFF]) for i in range(2)]
    HT = [P.sbuf("ht%d" % i, [128, 3, 128]) for i in range(2)]
    YT = [P.sbuf("yt%d" % i, [128, D]) for i in range(2)]
    PSBl = env["PSB"]
    SUBS = SLOT_R // 128
    NJ = NSLOT_T * SUBS

    def load_w(j):
        wi, wo = WI[j % 3], WO[j % 3]
        for m in range(3):
            P.dma("pool", lambda m=m: pool.indirect_dma_start(
                out=wi[:].rearrange("p k n -> p (k n)")[:, m * 2048:(m + 1) * 2048].bitcast(F32R), out_offset=None,
                in_=w_in_rows, in_offset=bass.IndirectOffsetOnAxis(ap=EJ3[:, m, j:j + 1], axis=0),
                bounds_check=BREG3, oob_is_err=False), reads=[EJ3], writes=[wi], accum=(m > 0))
        for m in range(2):
            P.dma("pool", lambda m=m: pool.indirect_dma_start(
                out=wo[:].rearrange("p k n -> p (k n)")[:, m * 1536:(m + 1) * 1536].bitcast(F32R), out_offset=None,
                in_=w_out_rows, in_offset=bass.IndirectOffsetOnAxis(ap=EJ2[:, m, j:j + 1], axis=0),
                bounds_check=BREG2, oob_is_err=False), reads=[EJ2], writes=[wo], accum=(m > 0))

    def load_x(jj):
        xs = XS[jj % 3]
        P.dma("sp", lambda: sp.dma_start(out=xs[:], in_=xs_d[jj * 128:(jj + 1) * 128, :]), reads=[xs_d], writes=[xs])

    def stC(jj):
        xs, xt8 = XS[jj % 3], XT8[jj % 2]
        tb = [PSBl[0], PSBl[1]]
        for kc in range(8):
            bb = tb[kc // 4]
            P.op("pe", lambda kc=kc, bb=bb: pe.transpose(
                bb[:, (kc % 4) * 128:(kc % 4 + 1) * 128], xs[:].rearrange("s (p k) -> s k p", k=8)[:, kc, :], ident),
                reads=[xs, CST], writes=[bb], accum=(kc % 4 > 0))
        P.op("act", lambda: act.copy(out=xt8[:, 0:4, :].rearrange("p a b -> p (a b)").bitcast(F32R), in_=tb[0][:]),
             reads=[tb[0]], writes=[xt8])
        P.op("dve", lambda: dve.tensor_copy(out=xt8[:, 4:8, :].rearrange("p a b -> p (a b)").bitcast(F32R), in_=tb[1][:]),
             reads=[tb[1]], writes=[xt8], accum=True)

    def stA(jj):
        xt8, hs = XT8[jj % 2], HS[jj % 2]
        wi = WI[(jj // SUBS) % 3]
        gb, ub = PSBl[2], PSBl[3]
        for kc in range(8):
            P.op("pe", lambda kc=kc: pe.matmul(gb[:, 0:DFF], lhsT=xt8[:, kc, :].bitcast(F32R),
                                               rhs=wi[:, kc, 0:DFF].bitcast(F32R), start=(kc == 0), stop=(kc == 7)),
                 reads=[xt8, wi], writes=[gb], accum=(kc > 0))
        for kc in range(8):
            P.op("pe", lambda kc=kc: pe.matmul(ub[:, 0:DFF], lhsT=xt8[:, kc, :].bitcast(F32R),
                                               rhs=wi[:, kc, DFF:2 * DFF].bitcast(F32R), start=(kc == 0), stop=(kc == 7)),
                 reads=[xt8, wi], writes=[ub], accum=(kc > 0))
        P.op("act", lambda: act.activation(out=SG[:], in_=gb[:, 0:DFF], func=ACT.Silu), reads=[gb], writes=[SG])
        P.op("dve", lambda: dve.tensor_tensor(out=hs[:], in0=ub[:, 0:DFF], in1=SG[:], op=ALU.mult),
             reads=[ub, SG], writes=[hs])

    def stB(jj):
        hs, ht = HS[jj % 2], HT[jj % 2]
        hb = PSBl[4]
        for fc in range(3):
            P.op("pe", lambda fc=fc: pe.transpose(hb[:, fc * 128:(fc + 1) * 128],
                                                  hs[:].rearrange("s (p k) -> s k p", k=3)[:, fc, :], ident),
                 reads=[hs, CST], writes=[hb], accum=(fc > 0))
        P.op("act", lambda: act.copy(out=ht[:].rearrange("p a b -> p (a b)").bitcast(F32R), in_=hb[:, 0:384]),
             reads=[hb], writes=[ht])

    def stD(jj):
        ht, yt = HT[jj % 2], YT[jj % 2]
        wo = WO[(jj // SUBS) % 3]
        yb = [PSBl[5], PSBl[6]]
        for hh in range(2):
            for fc in range(3):
                P.op("pe", lambda hh=hh, fc=fc: pe.matmul(
                    yb[hh][:], lhsT=ht[:, fc, :].bitcast(F32R), rhs=wo[:, fc, hh * 512:(hh + 1) * 512].bitcast(F32R),
                    start=(fc == 0), stop=(fc == 2)), reads=[ht, wo], writes=[yb[hh]], accum=(fc > 0))
        P.op("act", lambda: act.copy(out=yt[:, 0:512], in_=yb[0][:]), reads=[yb[0]], writes=[yt])
        P.op("dve", lambda: dve.tensor_copy(out=yt[:, 512:1024], in_=yb[1][:]), reads=[yb[1]], writes=[yt], accum=True)
        P.dma("sp", lambda: sp.dma_start(out=ys_d[jj * 128:(jj + 1) * 128, :], in_=yt[:]),
              reads=[yt], writes=[ys_d], sembuf=yt)

    load_w(0)
    load_w(1)
    load_x(0)
    load_x(1)
    stC(0)
    for jj in range(NJ + 1):
        if jj + 2 < NJ:
            load_x(jj + 2)
        if jj < NJ:
            stA(jj)
        if jj >= 1:
            stB(jj - 1)
        if jj + 1 < NJ:
            stC(jj + 1)
        if jj >= 1:
            stD(jj - 1)
        if jj % SUBS == 0 and jj // SUBS + 2 < NSLOT_T:
            load_w(jj // SUBS + 2)
    P.release(mB)

    mC = P.mark()
    XT = [P.sbuf("cxt%d" % i, [128, D]) for i in range(2)]
    Y0 = [P.sbuf("cy0%d" % i, [128, D]) for i in range(2)]
    Y1 = [P.sbuf("cy1%d" % i, [128, D]) for i in range(2)]
    for i in range(NT):
        xt, y0, y1 = XT[i % 2], Y0[i % 2], Y1[i % 2]
        P.dma("act", lambda xt=xt, i=i: act.dma_start(out=xt[:], in_=out[i * 128:(i + 1) * 128, :]),
              reads=[XD[i]], writes=[xt])
        for yy, SL in ((y0, SL0), (y1, SL1)):
            P.dma("pool", lambda yy=yy, SL=SL, i=i: pool.indirect_dma_start(
                out=yy[:, :], out_offset=None, in_=ys_d[:, :],
                in_offset=bass.IndirectOffsetOnAxis(ap=SL[:, i:i + 1], axis=0), bounds_check=BREG, oob_is_err=False),
                reads=[ys_d, SL], writes=[yy])
        P.op("dve", lambda y0=y0, i=i: dve.tensor_scalar(out=y0[:], in0=y0[:], scalar1=W0[:, i:i + 1], scalar2=None,
                                                         op0=ALU.mult), reads=[y0, W0], writes=[y0])
        P.op("dve", lambda y0=y0, y1=y1, i=i: dve.scalar_tensor_tensor(
            out=y0[:], in0=y1[:], scalar=W1[:, i:i + 1], in1=y0[:], op0=ALU.mult, op1=ALU.add),
            reads=[y0, y1, W1], writes=[y0])
        P.op("dve", lambda y0=y0: dve.tensor_tensor(out=y0[:], in0=y0[:], in1=G2, op=ALU.mult),
             reads=[y0, MODB], writes=[y0])
        P.op("dve", lambda y0=y0, xt=xt: dve.tensor_tensor(out=y0[:], in0=y0[:], in1=xt[:], op=ALU.add),
             reads=[y0, xt], writes=[y0])
        P.dma("sp", lambda y0=y0, i=i: sp.dma_start(out=out[i * 128:(i + 1) * 128, :], in_=y0[:]),
              reads=[y0], writes=[XD[i]], sembuf=y0)
    P.release(mC)
    P.release(mR)


def gla_layer(P, nc, L, j2, env, gstop=99):
    pe, act, dve, pool, sp = nc.tensor, nc.scalar, nc.vector, nc.gpsimd, nc.sync
    C, ident, ones, nextbank, xsrc, rstd_of = [env[k] for k in ("C", "ident", "ones", "nextbank", "xsrc", "rstd_of")]
    A1, SH1, G1, MODB, CST = [env[k] for k in ("A1", "SH1", "G1", "MODB", "CST")]
    out, XD = env["out"], env["XD"]
    gla_w_in, gla_w_gate, gla_b_gate, gla_norm_g, gla_w_out = [
        env[k] for k in ("gla_w_in", "gla_w_gate", "gla_b_gate", "gla_norm_g", "gla_w_out")]
    SC = 128 ** -0.5
    GT = 1.0 / 16.0

    WIN = P.sbuf("g_win", [128, 8, GIN], BF16)
    WOUT = P.sbuf("g_wout", [128, 8, D], BF16)
    IDB = P.sbuf("g_idb", [128, 128], BF16)
    WG = P.sbuf("g_wg", [16, 512])
    BG = P.sbuf("g_bg", [1, 512])
    GN = P.sbuf("g_gn", [128, 256])
    ms = P.mark()
    STG = [P.sbuf("g_stg%d" % i, [128, GIN]) for i in range(2)]
    for kc in range(8):
        st = STG[kc % 2]
        P.dma("sp", lambda st=st, kc=kc: sp.dma_start(out=st[:], in_=gla_w_in[j2, kc * 128:(kc + 1) * 128, :]), writes=[st])
        if kc % 2 == 0:
            P.op("act", lambda st=st, kc=kc: act.copy(out=WIN[:, kc, :], in_=st[:]), reads=[st], writes=[WIN], accum=True)
        else:
            P.op("dve", lambda st=st, kc=kc: dve.tensor_copy(out=WIN[:, kc, :], in_=st[:]), reads=[st], writes=[WIN], accum=True)
    for kc in range(8):
        st = STG[kc % 2]
        P.dma("sp", lambda st=st, kc=kc: sp.dma_start(out=st[:, 0:D], in_=gla_w_out[j2, kc * 128:(kc + 1) * 128, :]), writes=[st])
        if kc % 2 == 0:
            P.op("act", lambda st=st, kc=kc: act.copy(out=WOUT[:, kc, :], in_=st[:, 0:D]), reads=[st], writes=[WOUT], accum=True)
        else:
            P.op("dve", lambda st=st, kc=kc: dve.tensor_copy(out=WOUT[:, kc, :], in_=st[:, 0:D]), reads=[st], writes=[WOUT], accum=True)
    P.release(ms)
    P.op("dve", lambda: dve.tensor_copy(out=IDB[:], in_=ident), reads=[CST], writes=[IDB])
    P.dma("sp", lambda: sp.dma_start(out=WG[:], in_=gla_w_gate[j2]), writes=[WG])
    P.dma("sp", lambda: sp.dma_start(out=BG[:], in_=gla_b_gate[j2:j2 + 1, :]), writes=[BG])
    P.dma("sp", lambda: sp.dma_start(out=GN[:], in_=gla_norm_g[j2:j2 + 1, :].partition_broadcast(128)), writes=[GN])

    XT = [P.sbuf("g_xt%d" % i, [128, D]) for i in range(2)]
    H = [P.sbuf("g_h%d" % i, [128, D]) for i in range(2)]
    HB = P.sbuf("g_hb", [128, D], BF16)
    HT = P.sbuf("g_ht", [128, 8, 128], BF16)
    SSQ = P.sbuf("g_ssq", [128, 1])
    RSTD = P.sbuf("g_rstd", [128, 1])
    QK = P.sbuf("g_qk", [128, D])
    V = P.sbuf("g_v", [128, D])
    SR = P.sbuf("g_sr", [128, D])
    Z = P.sbuf("g_z", [128, 16])
    ZT = P.sbuf("g_zt", [16, 128])
    G = P.sbuf("g_g", [128, 512])
    EB = P.sbuf("g_eb", [128, 4, 128])
    ENB = P.sbuf("g_enb", [128, 4, 128])
    EREV = P.sbuf("g_erev", [128, 512])
    QEZ = P.sbuf("g_qez", [128, 4, 2, 128])
    KE = P.sbuf("g_ke", [128, 4, 128])
    KD0 = P.sbuf("g_kd0", [128, 512])
    KD1 = P.sbuf("g_kd1", [128, 512])
    SA = [P.sbuf("g_sa%d" % h, [128, 256]) for h in range(4)]
    SB = [P.sbuf("g_sb%d" % h, [128, 256]) for h in range(4)]
    SN = [P.sbuf("g_sn%d" % h, [128, 256]) for h in range(4)]
    ST4 = P.sbuf("g_st4", [128, 4, 128])
    SS4 = P.sbuf("g_ss4", [128, 4])
    RS4 = P.sbuf("g_rs4", [128, 4])
    JK = P.sbuf("g_jk", [128, 256])

    P.op("dve", lambda: dve.memset(QEZ[:], 0.0), writes=[QEZ])
    P.op("dve", lambda: dve.memset(KD0[:], 0.0), writes=[KD0])
    P.op("dve", lambda: dve.memset(KD1[:], 0.0), writes=[KD1])
    for h in range(4):
        P.op("dve", lambda h=h: dve.memset(SA[h][:], 0.0), writes=[SA[h]])

    for i in range(NT):
        xt, hh_ = XT[i % 2], H[i % 2]
        sb_, sa_ = xsrc(L, i)
        P.dma("act", lambda xt=xt, sa_=sa_: act.dma_start(out=xt[:], in_=sa_), reads=[sb_], writes=[xt])
        rstd_of((xt, xt[:]), hh_, SSQ, RSTD, D)
        P.op("dve", lambda xt=xt, hh_=hh_: dve.scalar_tensor_tensor(
            out=hh_[:], in0=xt[:], scalar=RSTD[:, 0:1], in1=A1, op0=ALU.mult, op1=ALU.mult),
            reads=[xt, RSTD, MODB], writes=[hh_])
        P.op("dve", lambda hh_=hh_: dve.tensor_tensor(out=HB[:], in0=hh_[:], in1=SH1, op=ALU.add),
             reads=[hh_, MODB], writes=[HB])
        tb = nextbank()
        tbb = tb[:].bitcast(BF16)
        for kc in range(8):
            P.op("pe", lambda kc=kc, tbb=tbb: pe.transpose(tbb[:, kc * 128:(kc + 1) * 128], HB[:, kc * 128:(kc + 1) * 128], IDB[:]),
                 reads=[HB, IDB], writes=[tb], accum=(kc > 0))
        P.op("act", lambda tbb=tbb: act.copy(out=HT[:].rearrange("p a b -> p (a b)"), in_=tbb), reads=[tb], writes=[HT])

        def proj(n0, n):
            b = nextbank()
            for kc in range(8):
                P.op("pe", lambda kc=kc, b=b: pe.matmul(b[:, 0:n], lhsT=HT[:, kc, :], rhs=WIN[:, kc, n0:n0 + n],
                                                         start=(kc == 0), stop=(kc == 7)),
                     reads=[HT, WIN], writes=[b], accum=(kc > 0))
            return b
        b = proj(0, 512)
        P.op("dve", lambda b=b: dve.tensor_copy(out=QK[:, 0:512], in_=b[:]), reads=[b], writes=[QK])
        b = proj(512, 512)
        P.op("act", lambda b=b: act.copy(out=QK[:, 512:1024], in_=b[:]), reads=[b], writes=[QK], accum=True)
        b = proj(1024, 512)
        P.op("dve", lambda b=b: dve.tensor_copy(out=V[:, 0:512], in_=b[:]), reads=[b], writes=[V])
        b = proj(1536, 512)
        P.op("act", lambda b=b: act.copy(out=V[:, 512:1024], in_=b[:]), reads=[b], writes=[V], accum=True)
        b = proj(2048, 512)
        P.op("act", lambda b=b: act.activation(out=SR[:, 0:512], in_=b[:], func=ACT.Silu), reads=[b], writes=[SR])
        b = proj(2560, 512)
        P.op("act", lambda b=b: act.activation(out=SR[:, 512:1024], in_=b[:], func=ACT.Silu), reads=[b], writes=[SR], accum=True)
        b = proj(3072, 16)
        P.op("dve", lambda b=b: dve.tensor_copy(out=Z[:], in_=b[:, 0:16]), reads=[b], writes=[Z])
        P.op("dve", lambda: dve.tensor_tensor(out=SR[:].rearrange("p (h v) -> p h v", h=4), in0=SR[:].rearrange("p (h v) -> p h v", h=4),
                                              in1=GN[:].unsqueeze(1).broadcast_to([128, 4, 256]), op=ALU.mult),
             reads=[SR, GN], writes=[SR])
        if gstop < 2:
            continue
        b = nextbank()
        P.op("pe", lambda b=b: pe.transpose(b[0:16, 0:128], Z[:], ident), reads=[Z, CST], writes=[b])
        P.op("act", lambda b=b: act.copy(out=ZT[:], in_=b[0:16, 0:128]), reads=[b], writes=[ZT])
        b = nextbank()
        P.op("pe", lambda b=b: pe.matmul(b[:], lhsT=ZT[:], rhs=WG[:], start=True, stop=False), reads=[ZT, WG], writes=[b])
        P.op("pe", lambda b=b: pe.matmul(b[:], lhsT=ones[0:1, :], rhs=BG[0:1, :], start=False, stop=True),
             reads=[CST, BG], writes=[b], accum=True)
        P.op("act", lambda b=b: act.activation(out=G[:], in_=b[:], func=ACT.Exp, scale=-1.0), reads=[b], writes=[G])
        P.op("act", lambda: act.activation(out=G[:], in_=G[:], func=ACT.Ln, bias=1.0), reads=[G], writes=[G])
        b = nextbank()
        for h in range(4):
            P.op("pe", lambda h=h, b=b: pe.matmul(b[:, h * 128:(h + 1) * 128], lhsT=G[:, h * 128:(h + 1) * 128], rhs=C("tri2"),
                                                   start=True, stop=True), reads=[G, CST], writes=[b], accum=(h > 0))
        P.op("act", lambda b=b: act.activation(out=EB[:].rearrange("p a b -> p (a b)"), in_=b[:], func=ACT.Exp, scale=-GT),
             reads=[b], writes=[EB])
        P.op("act", lambda b=b: act.activation(out=ENB[:].rearrange("p a b -> p (a b)"), in_=b[:], func=ACT.Exp, scale=GT),
             reads=[b], writes=[ENB])
        b = nextbank()
        P.op("pe", lambda b=b: pe.matmul(b[:], lhsT=C("triu2"), rhs=G[:], start=True, stop=True), reads=[G, CST], writes=[b])
        P.op("act", lambda b=b: act.activation(out=EREV[:], in_=b[:], func=ACT.Exp, scale=-GT), reads=[b], writes=[EREV])
        if gstop < 3:
            continue
        qb, kb = nextbank(), nextbank()
        for h in range(4):
            P.op("pe", lambda h=h, qb=qb: pe.transpose(qb[:, h * 128:(h + 1) * 128], QK[:, h * 128:(h + 1) * 128], ident),
                 reads=[QK, CST], writes=[qb], accum=(h > 0))
        for h in range(4):
            P.op("pe", lambda h=h, kb=kb: pe.transpose(kb[:, h * 128:(h + 1) * 128], QK[:, 512 + h * 128:512 + (h + 1) * 128], ident),
                 reads=[QK, CST], writes=[kb], accum=(h > 0))
        qb3 = qb[:].rearrange("p (h t) -> p h t", h=4)
        for cc in range(2):
            P.op("dve", lambda cc=cc, qb3=qb3: dve.scalar_tensor_tensor(
                out=QEZ[:, :, cc, cc * 64:(cc + 1) * 64], in0=qb3[:, :, cc * 64:(cc + 1) * 64], scalar=SC,
                in1=EB[:, :, cc * 64:(cc + 1) * 64], op0=ALU.mult, op1=ALU.mult),
                reads=[qb, EB], writes=[QEZ], accum=(cc > 0))
        P.op("dve", lambda kb=kb: dve.tensor_tensor(out=KE[:].rearrange("p a b -> p (a b)"), in0=kb[:],
                                                    in1=ENB[:].rearrange("p a b -> p (a b)"), op=ALU.mult),
             reads=[kb, ENB], writes=[KE])
        P.op("dve", lambda: dve.tensor_tensor(out=KD0[0:64, :], in0=QK[0:64, 512:1024], in1=EREV[0:64, :], op=ALU.mult),
             reads=[QK, EREV], writes=[KD0])
        P.op("dve", lambda: dve.tensor_tensor(out=KD1[64:128, :], in0=QK[64:128, 512:1024], in1=EREV[64:128, :], op=ALU.mult),
             reads=[QK, EREV], writes=[KD1])
        if gstop < 4:
            continue
        PSBl = env["PSB"]
        ob = [PSBl[0], PSBl[1]]
        scb = PSBl[2]
        sub_ = [PSBl[3], PSBl[4]]
        su2b = [PSBl[5], PSBl[6]]
        S0 = SA if i % 2 == 0 else SN
        S2 = SN if i % 2 == 0 else SA
        for h in range(4):
            o_sc = scb[:, h * 128:(h + 1) * 128]
            P.op("pe", lambda h=h, o_sc=o_sc: pe.matmul(o_sc, lhsT=KE[:, h, :], rhs=QEZ[:, h, 0, :], start=True, stop=False),
                 reads=[KE, QEZ], writes=[scb], accum=(h > 0))
            P.op("pe", lambda h=h, o_sc=o_sc: pe.matmul(o_sc, lhsT=KE[:, h, :], rhs=QEZ[:, h, 1, :], start=False, stop=True),
                 reads=[KE, QEZ], writes=[scb], accum=True)
        for h in range(4):
            vh = V[:, h * 256:(h + 1) * 256]
            P.op("pe", lambda h=h, vh=vh: pe.matmul(sub_[h // 2][:, (h % 2) * 256:(h % 2 + 1) * 256],
                                                    lhsT=KD0[:, h * 128:(h + 1) * 128], rhs=vh, start=True, stop=True),
                 reads=[KD0, V], writes=[sub_[h // 2]], accum=(h % 2 > 0))
        for h in range(4):
            vh = V[:, h * 256:(h + 1) * 256]
            P.op("pe", lambda h=h, vh=vh: pe.matmul(su2b[h // 2][:, (h % 2) * 256:(h % 2 + 1) * 256],
                                                    lhsT=KD1[:, h * 128:(h + 1) * 128], rhs=vh, start=True, stop=True),
                 reads=[KD1, V], writes=[su2b[h // 2]], accum=(h % 2 > 0))
        P.op("dve", lambda: dve.tensor_tensor(out=ST4[:], in0=scb[:].rearrange("p (h t) -> p h t", h=4),
                                              in1=C("tri2").unsqueeze(1).broadcast_to([128, 4, 128]), op=ALU.mult),
             reads=[scb, CST], writes=[ST4])
        for h in range(4):
            P.op("dve", lambda h=h: dve.scalar_tensor_tensor(
                out=SB[h][:], in0=S0[h][:], scalar=EB[:, h, 63:64], in1=sub_[h // 2][:, (h % 2) * 256:(h % 2 + 1) * 256],
                op0=ALU.mult, op1=ALU.add), reads=[S0[h], EB, sub_[h // 2]], writes=[SB[h]])
        for h in range(4):
            P.op("dve", lambda h=h: dve.scalar_tensor_tensor(
                out=S2[h][:], in0=SB[h][:], scalar=EB[:, h, 127:128], in1=su2b[h // 2][:, (h % 2) * 256:(h % 2 + 1) * 256],
                op0=ALU.mult, op1=ALU.add), reads=[SB[h], EB, su2b[h // 2]], writes=[S2[h]])
        for h in range(4):
            vh = V[:, h * 256:(h + 1) * 256]
            o_ap = ob[h // 2][:, (h % 2) * 256:(h % 2 + 1) * 256]
            P.op("pe", lambda h=h, vh=vh, o_ap=o_ap: pe.matmul(o_ap, lhsT=ST4[:, h, :], rhs=vh, start=True, stop=False),
                 reads=[ST4, V], writes=[ob[h // 2]], accum=(h % 2 > 0))
            P.op("pe", lambda h=h, o_ap=o_ap: pe.matmul(o_ap, lhsT=QEZ[:, h, 0, :], rhs=S0[h][:], start=False, stop=False),
                 reads=[QEZ, S0[h]], writes=[ob[h // 2]], accum=True)
            P.op("pe", lambda h=h, o_ap=o_ap: pe.matmul(o_ap, lhsT=QEZ[:, h, 1, :], rhs=SB[h][:], start=False, stop=True),
                 reads=[QEZ, SB[h]], writes=[ob[h // 2]], accum=True)
        if gstop < 5:
            continue
        OS = QK
        P.op("act", lambda: act.copy(out=OS[:, 0:512], in_=ob[0][:]), reads=[ob[0]], writes=[OS])
        P.op("act", lambda: act.copy(out=OS[:, 512:1024], in_=ob[1][:]), reads=[ob[1]], writes=[OS], accum=True)
        for h in range(4):
            oh = OS[:, h * 256:(h + 1) * 256]
            P.op("dve", lambda h=h, oh=oh: dve.scalar_tensor_tensor(out=JK[:], in0=oh, scalar=1.0, in1=oh, op0=ALU.mult,
                                                                    op1=ALU.mult, accum_out=SS4[:, h:h + 1]),
                 reads=[OS], writes=[JK, SS4])
        P.op("act", lambda: act.activation(out=RS4[:], in_=SS4[:], func=ACT.Sqrt, scale=1.0 / 256, bias=EPS),
             reads=[SS4], writes=[RS4])
        P.op("dve", lambda: dve.reciprocal(out=RS4[:], in_=RS4[:]), reads=[RS4], writes=[RS4])
        for h in range(4):
            P.op("dve", lambda h=h: dve.scalar_tensor_tensor(
                out=V[:, h * 256:(h + 1) * 256], in0=OS[:, h * 256:(h + 1) * 256], scalar=RS4[:, h:h + 1],
                in1=SR[:, h * 256:(h + 1) * 256], op0=ALU.mult, op1=ALU.mult),
                reads=[OS, RS4, SR], writes=[V], accum=(h > 0))
        P.op("act", lambda: act.copy(out=HB[:], in_=V[:]), reads=[V], writes=[HB])
        if gstop < 6:
            continue
        tb = nextbank()
        tbb = tb[:].bitcast(BF16)
        for kc in range(8):
            P.op("pe", lambda kc=kc, tbb=tbb: pe.transpose(tbb[:, kc * 128:(kc + 1) * 128], HB[:, kc * 128:(kc + 1) * 128], IDB[:]),
                 reads=[HB, IDB], writes=[tb], accum=(kc > 0))
        P.op("act", lambda tbb=tbb: act.copy(out=HT[:].rearrange("p a b -> p (a b)"), in_=tbb), reads=[tb], writes=[HT])
        yb = [nextbank(), nextbank()]
        for q in range(2):
            for kc in range(8):
                P.op("pe", lambda q=q, kc=kc: pe.matmul(yb[q][:], lhsT=HT[:, kc, :], rhs=WOUT[:, kc, q * 512:(q + 1) * 512],
                                                        start=(kc == 0), stop=(kc == 7)),
                     reads=[HT, WOUT], writes=[yb[q]], accum=(kc > 0))
        if gstop < 7:
            continue
        yo = hh_
        for q in range(2):
            sl = slice(q * 512, (q + 1) * 512)
            P.op("dve", lambda q=q, sl=sl, yo=yo: dve.tensor_tensor(out=yo[:, sl], in0=yb[q][:], in1=G1[:, sl], op=ALU.mult),
                 reads=[yb[q], MODB], writes=[yo], accum=(q > 0))
        P.op("dve", lambda yo=yo, xt=xt: dve.tensor_tensor(out=yo[:], in0=yo[:], in1=xt[:], op=ALU.add),
             reads=[yo, xt], writes=[yo])
        P.dma("sp", lambda yo=yo, i=i: sp.dma_start(out=out[i * 128:(i + 1) * 128, :], in_=yo[:]),
              reads=[yo], writes=[XD[i]], sembuf=yo)


_CACHE = {}


def kernel(**inputs):
    nl = DEPTH
    if "nc" not in _CACHE:
        _CACHE["nc"] = build(nl)
    nc = _CACHE["nc"]
    f = lambda k: np.ascontiguousarray(np.asarray(inputs[k], dtype=np.float32))
    shared = {k: f(k) for k in inputs if k not in ("x", "c")}
    shared["final_norm_g"] = shared["final_norm_g"].reshape(1, D)
    shared["consts"] = CONST_ARR
    x = f("x")
    c = f("c")
    in_maps = []
    for b in range(8):
        m = dict(shared)
        m["x"] = x[b]
        m["c"] = c[b:b + 1]
        in_maps.append(m)
    res = run_bass_kernel_spmd(nc, in_maps, core_ids=list(range(8)))
    return np.stack([r["out"] for r in res.results], axis=0).astype(np.float32)
```
